# Optimizing a Trainium2 kernel written in Bass

```python
import math
import jax
import jax.numpy as jnp
from jax import lax
import numpy as np

D_MODEL = 2048
BATCH = 1
SEQ = 16384
DEPTH = 2
DEC_BATCH = 8
DEC_SEQ = 16
PAST_LEN = 2048

CHUNK = 64
N_EVEN = (DEPTH + 1) // 2
N_ODD = DEPTH // 2
A_HEADS = 16
A_HEAD_DIM = 64
A_WIDTH = A_HEADS * A_HEAD_DIM
A_PAST_CHUNKS = 8
A_WINDOW = A_PAST_CHUNKS * CHUNK
A_BAND = A_WINDOW + CHUNK
REL_CLIP = 128
B_HEADS = 16
B_HEAD_DIM = 64
B_WIDTH = B_HEADS * B_HEAD_DIM
B_GROUPS = 2
B_STATE = 128
B_CONV = 4
B_CONV_DIM = B_WIDTH + 2 * B_GROUPS * B_STATE
SSD_CHUNK = CHUNK
C_WIDTH = D_MODEL
C_GROUP_CH = 16
C_GROUPS = C_WIDTH // C_GROUP_CH
C_STATE = 64
D_FF = 5632
N_EXPERTS = 8
TOP_K = 2
D_FF_EXPERT = 2816
ALPHA = (2.0 * DEPTH) ** 0.25
BETA = (8.0 * DEPTH) ** -0.25
LN_EPS = 1e-5
RMS_EPS = 1e-5
NEG_INF = -1e30
IN0_SPLITS = (A_WIDTH, 2 * A_WIDTH, 3 * A_WIDTH, 3 * A_WIDTH + B_WIDTH, 3 * A_WIDTH + B_WIDTH + B_CONV_DIM)
IN0_WIDTH = 3 * A_WIDTH + B_WIDTH + B_CONV_DIM + B_HEADS

kernel_name = 'hybrid_streaming_encoder_step'


def layer_norm(x, g, b):
    xf = x.astype(jnp.float32)
    mu = jnp.mean(xf, axis=-1, keepdims=True)
    var = jnp.mean(jnp.square(xf - mu), axis=-1, keepdims=True)
    return ((xf - mu) * lax.rsqrt(var + LN_EPS) * g + b).astype(x.dtype)


def rms_norm(x, g):
    xf = x.astype(jnp.float32)
    return xf * lax.rsqrt(jnp.mean(jnp.square(xf), axis=-1, keepdims=True) + RMS_EPS) * g


def adaln(c, w, b):
    mod = jax.nn.silu(c) @ w + b
    shift, scale, gate = jnp.split(mod[:, None, :], 3, axis=-1)
    return shift, scale, gate


def swiglu(h, w_up, w_down):
    g, u = jnp.split(h @ w_up, 2, axis=-1)
    return (jax.nn.silu(g) * u) @ w_down


def band_allowed(q_pos, k_pos):
    qc = q_pos[:, None] // CHUNK
    kc = k_pos[None, :] // CHUNK
    return (kc <= qc) & (kc >= qc - A_PAST_CHUNKS) & (k_pos[None, :] >= 0)


def chunk_attend(q, k, v, q_pos, k_pos, rel_bias):
    s = jnp.einsum('bqhd,bkhd->bhqk', q, k).astype(jnp.float32) * (A_HEAD_DIM ** -0.5)
    rel = jnp.clip(q_pos[:, None] - k_pos[None, :], -REL_CLIP, REL_CLIP) + REL_CLIP
    s = s + rel_bias[:, rel].astype(jnp.float32)[None]
    s = jnp.where(band_allowed(q_pos, k_pos)[None, None], s, NEG_INF)
    p = jax.nn.softmax(s, axis=-1).astype(v.dtype)
    return jnp.einsum('bhqk,bkhd->bqhd', p, v)


def band_attention_prompt(q, k, v, rel_bias):
    bsz, L, H, dh = q.shape
    n_chunks = L // CHUNK
    pad = jnp.zeros((bsz, A_WINDOW, H, dh), k.dtype)
    kp = jnp.concatenate([pad, k], axis=1)
    vp = jnp.concatenate([pad, v], axis=1)

    def one_chunk(ci):
        start = ci * CHUNK
        qc = lax.dynamic_slice_in_dim(q, start, CHUNK, axis=1)
        kb = lax.dynamic_slice_in_dim(kp, start, A_BAND, axis=1)
        vb = lax.dynamic_slice_in_dim(vp, start, A_BAND, axis=1)
        q_pos = start + jnp.arange(CHUNK)
        k_pos = start - A_WINDOW + jnp.arange(A_BAND)
        return chunk_attend(qc, kb, vb, q_pos, k_pos, rel_bias)

    out = lax.map(one_chunk, jnp.arange(n_chunks))
    return jnp.transpose(out, (1, 0, 2, 3, 4)).reshape(bsz, L, H, dh)


def band_attention_sample(q, k, v, k_cache, v_cache, rel_bias):
    Lq = q.shape[1]
    W = k_cache.shape[1]
    kk = jnp.concatenate([k_cache.astype(k.dtype), k], axis=1)
    vv = jnp.concatenate([v_cache.astype(v.dtype), v], axis=1)
    q_pos = PAST_LEN + jnp.arange(Lq)
    k_pos = PAST_LEN - W + jnp.arange(W + Lq)
    return chunk_attend(q, kk, vv, q_pos, k_pos, rel_bias)


def causal_dwconv(x, buf, w, b):
    L = x.shape[1]
    xp = jnp.concatenate([buf.astype(x.dtype), x], axis=1)
    y = b + xp[:, 0:L] * w[0]
    for tap in range(1, B_CONV):
        y = y + xp[:, tap:tap + L] * w[tap]
    return y, xp[:, L:]


def ssd_scan(x, dt, A, Bm, Cm, h0, q_len):
    bsz, L, H, P = x.shape
    N = Bm.shape[-1]
    nc = L // q_len
    xs = x.astype(jnp.float32).reshape(bsz, nc, q_len, H, P)
    dts = dt.reshape(bsz, nc, q_len, H)
    Bs = Bm.astype(jnp.float32).reshape(bsz, nc, q_len, H, N)
    Cs = Cm.astype(jnp.float32).reshape(bsz, nc, q_len, H, N)
    acs = jnp.cumsum(dts * A, axis=2)
    diff = acs[:, :, :, None, :] - acs[:, :, None, :, :]
    tril = jnp.tril(jnp.ones((q_len, q_len), dtype=bool))[None, None, :, :, None]
    decay = jnp.exp(jnp.where(tril, diff, -jnp.inf))
    xdt = xs * dts[..., None]
    scores = jnp.einsum('bcihn,bcjhn->bcijh', Cs, Bs) * decay
    y_diag = jnp.einsum('bcijh,bcjhp->bcihp', scores, xdt)
    to_end = jnp.exp(acs[:, :, -1:, :] - acs)
    chunk_states = jnp.einsum('bcjhn,bcjhp->bchpn', Bs * to_end[..., None], xdt)
    chunk_decay = jnp.exp(acs[:, :, -1, :])

    def step(h, inp):
        s_c, d_c = inp
        return h * d_c[:, :, None, None] + s_c, h

    h_final, h_in = lax.scan(step, h0.astype(jnp.float32),
                             (jnp.moveaxis(chunk_states, 1, 0), jnp.moveaxis(chunk_decay, 1, 0)))
    h_in = jnp.moveaxis(h_in, 0, 1)
    y_off = jnp.einsum('bcihn,bchpn->bcihp', Cs, h_in) * jnp.exp(acs)[..., None]
    return (y_diag + y_off).reshape(bsz, L, H, P), h_final


def ssd_mixer(z, xbc, dt_raw, conv_buf, h0, conv_w, conv_b, dt_bias, a_log, d_skip, norm_g, q_len):
    bsz, L, _ = z.shape
    xbc, conv_new = causal_dwconv(xbc, conv_buf, conv_w, conv_b)
    xbc = jax.nn.silu(xbc)
    xs, Bm, Cm = jnp.split(xbc, [B_WIDTH, B_WIDTH + B_GROUPS * B_STATE], axis=-1)
    xs = xs.reshape(bsz, L, B_HEADS, B_HEAD_DIM)
    rep = B_HEADS // B_GROUPS
    Bm = jnp.repeat(Bm.reshape(bsz, L, B_GROUPS, B_STATE), rep, axis=2)
    Cm = jnp.repeat(Cm.reshape(bsz, L, B_GROUPS, B_STATE), rep, axis=2)
    dt = jax.nn.softplus(dt_raw.astype(jnp.float32) + dt_bias.astype(jnp.float32))
    A = -jnp.exp(a_log.astype(jnp.float32))
    y, h_final = ssd_scan(xs, dt, A, Bm, Cm, h0, q_len)
    y = y + d_skip.astype(jnp.float32)[:, None] * xs.astype(jnp.float32)
    y = y.reshape(bsz, L, B_WIDTH) * jax.nn.silu(z.astype(jnp.float32))
    return rms_norm(y, norm_g).astype(z.dtype), conv_new, h_final


def attn_ssd_mixer(h, k_cache, v_cache, conv_buf, ssm_h0, w_in, w_out, rel_bias,
                   conv_w, conv_b, dt_bias, a_log, d_skip, norm_g):
    bsz, L, _ = h.shape
    q, k, v, z, xbc, dt_raw = jnp.split(h @ w_in, IN0_SPLITS, axis=-1)
    q = q.reshape(bsz, L, A_HEADS, A_HEAD_DIM)
    k = k.reshape(bsz, L, A_HEADS, A_HEAD_DIM)
    v = v.reshape(bsz, L, A_HEADS, A_HEAD_DIM)
    if k_cache is None:
        att = band_attention_prompt(q, k, v, rel_bias)
        keep = min(A_WINDOW, L)
        k_new, v_new = k[:, L - keep:], v[:, L - keep:]
        q_len = SSD_CHUNK
    else:
        att = band_attention_sample(q, k, v, k_cache, v_cache, rel_bias)
        k_new, v_new = k, v
        q_len = L
    y_ssd, conv_new, h_final = ssd_mixer(z, xbc, dt_raw, conv_buf, ssm_h0, conv_w, conv_b,
                                         dt_bias, a_log, d_skip, norm_g, q_len)
    out = jnp.concatenate([att.reshape(bsz, L, A_WIDTH), y_ssd], axis=-1) @ w_out
    return out, k_new, v_new, conv_new, h_final


def complex_affine_combine(e1, e2):
    a1r, a1i, b1r, b1i = e1
    a2r, a2i, b2r, b2i = e2
    return (a2r * a1r - a2i * a1i, a2r * a1i + a2i * a1r,
            a2r * b1r - a2i * b1i + b2r, a2r * b1i + a2i * b1r + b2i)


def s5_scan(u, h0_re, h0_im, lam_re, lam_im, log_step, b_re, b_im, c_re, c_im, d_skip):
    bsz, L, _ = u.shape
    f32 = jnp.float32
    uf = u.astype(f32).reshape(bsz, L, C_GROUPS, C_GROUP_CH)
    lre, lim = lam_re.astype(f32), lam_im.astype(f32)
    step = jnp.exp(log_step.astype(f32))[:, None]
    mag = jnp.exp(lre * step)
    ang = lim * step
    ab_re, ab_im = mag * jnp.cos(ang), mag * jnp.sin(ang)
    den = jnp.square(lre) + jnp.square(lim)
    f_re = ((ab_re - 1.0) * lre + ab_im * lim) / den
    f_im = (ab_im * lre - (ab_re - 1.0) * lim) / den
    br, bi = b_re.astype(f32), b_im.astype(f32)
    bb_re = f_re[..., None] * br - f_im[..., None] * bi
    bb_im = f_re[..., None] * bi + f_im[..., None] * br
    bu_re = jnp.einsum('blgc,gpc->blgp', uf, bb_re)
    bu_im = jnp.einsum('blgc,gpc->blgp', uf, bb_im)
    h_re, h_im = h0_re.astype(f32), h0_im.astype(f32)
    bu_re = bu_re.at[:, 0].add(ab_re * h_re - ab_im * h_im)
    bu_im = bu_im.at[:, 0].add(ab_re * h_im + ab_im * h_re)
    a_re = jnp.broadcast_to(ab_re, bu_re.shape)
    a_im = jnp.broadcast_to(ab_im, bu_im.shape)
    _, _, s_re, s_im = lax.associative_scan(complex_affine_combine, (a_re, a_im, bu_re, bu_im), axis=1)
    y = (jnp.einsum('blgp,gcp->blgc', s_re, c_re.astype(f32))
         - jnp.einsum('blgp,gcp->blgc', s_im, c_im.astype(f32))
         + d_skip.astype(f32).reshape(C_GROUPS, C_GROUP_CH) * uf)
    return y.reshape(bsz, L, C_WIDTH), s_re[:, -1], s_im[:, -1]


def s5_glu_mixer(h, h0_re, h0_im, w_in, lam_re, lam_im, log_step, b_re, b_im, c_re, c_im, d_skip, glu_w):
    y, s_re, s_im = s5_scan(h @ w_in, h0_re, h0_im, lam_re, lam_im, log_step, b_re, b_im, c_re, c_im, d_skip)
    y = jax.nn.gelu(y).astype(h.dtype)
    val, gt = jnp.split(y @ glu_w, 2, axis=-1)
    return val * jax.nn.sigmoid(gt), s_re, s_im


def moe_swiglu(h, router_w, router_b, w_up, w_down):
    bsz, L, D = h.shape
    t = h.reshape(bsz * L, D)
    logits = (t @ router_w).astype(jnp.float32) + router_b.astype(jnp.float32)
    top_val, top_idx = lax.top_k(logits, TOP_K)
    top_w = jax.nn.softmax(top_val, axis=-1)
    gates = jnp.sum(jax.nn.one_hot(top_idx, N_EXPERTS, dtype=jnp.float32) * top_w[..., None], axis=1)
    out = jnp.zeros((bsz * L, D), jnp.float32)
    for e in range(N_EXPERTS):
        out = out + gates[:, e:e + 1] * swiglu(t, w_up[e], w_down[e]).astype(jnp.float32)
    return out.astype(h.dtype).reshape(bsz, L, D)


def run_trunk(x, c, k_cache, v_cache, conv_cache, ssm_cache, s5_re_cache, s5_im_cache,
              ada_w, ada_b, ln_g, ln_b,
              w_in0, w_out0, rel_bias, conv_w, conv_b, dt_bias, a_log, ssd_d, ssd_norm_g,
              ffn_w_up, ffn_w_down,
              w_in1, s5_lam_re, s5_lam_im, s5_log_step, s5_b_re, s5_b_im, s5_c_re, s5_c_im, s5_d, glu_w,
              router_w, router_b, moe_w_up, moe_w_down):
    prompt = k_cache is None
    bsz = x.shape[0]
    new_k, new_v, new_conv, new_ssm, new_re, new_im = [], [], [], [], [], []
    for layer in range(DEPTH):
        i = layer // 2
        shift, scale, gate = adaln(c, ada_w[layer, 0], ada_b[layer, 0])
        h = x * (1.0 + scale) + shift
        if layer % 2 == 0:
            if prompt:
                kc, vc = None, None
                conv_buf = jnp.zeros((bsz, B_CONV - 1, B_CONV_DIM), x.dtype)
                h0 = jnp.zeros((bsz, B_HEADS, B_HEAD_DIM, B_STATE), jnp.float32)
            else:
                kc, vc, conv_buf, h0 = k_cache[i], v_cache[i], conv_cache[i], ssm_cache[i]
            out, kn, vn, cn, hn = attn_ssd_mixer(h, kc, vc, conv_buf, h0, w_in0[i], w_out0[i], rel_bias[i],
                                                 conv_w[i], conv_b[i], dt_bias[i], a_log[i], ssd_d[i],
                                                 ssd_norm_g[i])
            new_k.append(kn)
            new_v.append(vn)
            new_conv.append(cn)
            new_ssm.append(hn)
        else:
            if prompt:
                h0_re = jnp.zeros((bsz, C_GROUPS, C_STATE), jnp.float32)
                h0_im = jnp.zeros((bsz, C_GROUPS, C_STATE), jnp.float32)
            else:
                h0_re, h0_im = s5_re_cache[i], s5_im_cache[i]
            out, sr, si = s5_glu_mixer(h, h0_re, h0_im, w_in1[i], s5_lam_re[i], s5_lam_im[i], s5_log_step[i],
                                       s5_b_re[i], s5_b_im[i], s5_c_re[i], s5_c_im[i], s5_d[i], glu_w[i])
            new_re.append(sr)
            new_im.append(si)
        x = layer_norm(ALPHA * x + (1.0 + gate) * out, ln_g[layer, 0], ln_b[layer, 0])
        shift, scale, gate = adaln(c, ada_w[layer, 1], ada_b[layer, 1])
        h = x * (1.0 + scale) + shift
        if layer % 2 == 0:
            out = swiglu(h, ffn_w_up[i], ffn_w_down[i])
        else:
            out = moe_swiglu(h, router_w[i], router_b[i], moe_w_up[i], moe_w_down[i])
        x = layer_norm(ALPHA * x + (1.0 + gate) * out, ln_g[layer, 1], ln_b[layer, 1])
    return (x, jnp.stack(new_k), jnp.stack(new_v), jnp.stack(new_conv), jnp.stack(new_ssm),
            jnp.stack(new_re), jnp.stack(new_im))


def setup_inputs(seed: int = 0) -> dict:
    key = jax.random.key(seed)
    ks = iter(jax.random.split(key, 48))
    f32 = jnp.float32

    def nrm(shape, s):
        return jax.random.normal(next(ks), shape, f32) * s

    keep = min(A_WINDOW, PAST_LEN)
    x_prompt = nrm((BATCH, SEQ, D_MODEL), 1.0)
    x_sample = nrm((DEC_BATCH, DEC_SEQ, D_MODEL), 1.0)
    cache_attn_k = nrm((N_EVEN, DEC_BATCH, keep, A_HEADS, A_HEAD_DIM), 1.0)
    cache_attn_v = nrm((N_EVEN, DEC_BATCH, keep, A_HEADS, A_HEAD_DIM), 1.0)
    state_ssd_conv = nrm((N_EVEN, DEC_BATCH, B_CONV - 1, B_CONV_DIM), 1.0)
    state_ssd = nrm((N_EVEN, DEC_BATCH, B_HEADS, B_HEAD_DIM, B_STATE), 0.1)
    state_s5_re = nrm((N_ODD, DEC_BATCH, C_GROUPS, C_STATE), 0.5)
    state_s5_im = nrm((N_ODD, DEC_BATCH, C_GROUPS, C_STATE), 0.5)
    c_prompt = nrm((BATCH, D_MODEL), 1.0)
    c_sample = nrm((DEC_BATCH, D_MODEL), 1.0)
    ada_w = nrm((DEPTH, 2, D_MODEL, 3 * D_MODEL), 0.1 * D_MODEL ** -0.5)
    ada_b = nrm((DEPTH, 2, 3 * D_MODEL), 0.02)
    ln_g = 1.0 + nrm((DEPTH, 2, D_MODEL), 0.02)
    ln_b = nrm((DEPTH, 2, D_MODEL), 0.02)
    w_in0 = nrm((N_EVEN, D_MODEL, IN0_WIDTH), D_MODEL ** -0.5)
    w_out0 = nrm((N_EVEN, A_WIDTH + B_WIDTH, D_MODEL), BETA * (A_WIDTH + B_WIDTH) ** -0.5)
    rel_bias = nrm((N_EVEN, A_HEADS, 2 * REL_CLIP + 1), 0.2)
    conv_w = nrm((N_EVEN, B_CONV, B_CONV_DIM), B_CONV ** -0.5)
    conv_b = nrm((N_EVEN, B_CONV_DIM), 0.02)
    dt0 = jnp.exp(jax.random.uniform(next(ks), (N_EVEN, B_HEADS), f32, math.log(1e-3), math.log(1e-1)))
    dt_bias = dt0 + jnp.log(-jnp.expm1(-dt0))
    a_log = jnp.log(jax.random.uniform(next(ks), (N_EVEN, B_HEADS), f32, 1.0, 16.0))
    ssd_d = 1.0 + nrm((N_EVEN, B_HEADS), 0.1)
    ssd_norm_g = 1.0 + nrm((N_EVEN, B_WIDTH), 0.02)
    ffn_w_up = nrm((N_EVEN, D_MODEL, 2 * D_FF), D_MODEL ** -0.5)
    ffn_w_down = nrm((N_EVEN, D_FF, D_MODEL), BETA * D_FF ** -0.5)
    w_in1 = nrm((N_ODD, D_MODEL, C_WIDTH), D_MODEL ** -0.5)
    s5_lam_re = -0.5 + nrm((N_ODD, C_GROUPS, C_STATE), 0.01)
    s5_lam_im = math.pi * jnp.arange(C_STATE, dtype=f32)[None, None, :] + nrm((N_ODD, C_GROUPS, C_STATE), 0.01)
    s5_log_step = jax.random.uniform(next(ks), (N_ODD, C_GROUPS), f32, math.log(1e-3), math.log(1e-1))
    s5_b_re = nrm((N_ODD, C_GROUPS, C_STATE, C_GROUP_CH), (2 * C_GROUP_CH) ** -0.5)
    s5_b_im = nrm((N_ODD, C_GROUPS, C_STATE, C_GROUP_CH), (2 * C_GROUP_CH) ** -0.5)
    s5_c_re = nrm((N_ODD, C_GROUPS, C_GROUP_CH, C_STATE), (2 * C_STATE) ** -0.5)
    s5_c_im = nrm((N_ODD, C_GROUPS, C_GROUP_CH, C_STATE), (2 * C_STATE) ** -0.5)
    s5_d = nrm((N_ODD, C_WIDTH), 1.0)
    glu_w = jnp.concatenate([nrm((N_ODD, C_WIDTH, D_MODEL), BETA * C_WIDTH ** -0.5),
                             nrm((N_ODD, C_WIDTH, D_MODEL), C_WIDTH ** -0.5)], axis=-1)
    router_w = nrm((N_ODD, D_MODEL, N_EXPERTS), D_MODEL ** -0.5)
    router_b = nrm((N_ODD, N_EXPERTS), 0.01)
    moe_w_up = nrm((N_ODD, N_EXPERTS, D_MODEL, 2 * D_FF_EXPERT), D_MODEL ** -0.5)
    moe_w_down = nrm((N_ODD, N_EXPERTS, D_FF_EXPERT, D_MODEL), BETA * D_FF_EXPERT ** -0.5)
    return {'x_prompt': x_prompt, 'x_sample': x_sample,
            'cache_attn_k': cache_attn_k, 'cache_attn_v': cache_attn_v,
            'state_ssd_conv': state_ssd_conv, 'state_ssd': state_ssd,
            'state_s5_re': state_s5_re, 'state_s5_im': state_s5_im,
            'c_prompt': c_prompt, 'c_sample': c_sample,
            'ada_w': ada_w, 'ada_b': ada_b, 'ln_g': ln_g, 'ln_b': ln_b,
            'w_in0': w_in0, 'w_out0': w_out0, 'rel_bias': rel_bias, 'conv_w': conv_w, 'conv_b': conv_b,
            'dt_bias': dt_bias, 'a_log': a_log, 'ssd_d': ssd_d, 'ssd_norm_g': ssd_norm_g,
            'ffn_w_up': ffn_w_up, 'ffn_w_down': ffn_w_down,
            'w_in1': w_in1, 's5_lam_re': s5_lam_re, 's5_lam_im': s5_lam_im, 's5_log_step': s5_log_step,
            's5_b_re': s5_b_re, 's5_b_im': s5_b_im, 's5_c_re': s5_c_re, 's5_c_im': s5_c_im, 's5_d': s5_d,
            'glu_w': glu_w, 'router_w': router_w, 'router_b': router_b,
            'moe_w_up': moe_w_up, 'moe_w_down': moe_w_down}


def reference(x_prompt, x_sample, cache_attn_k, cache_attn_v, state_ssd_conv, state_ssd, state_s5_re, state_s5_im,
              c_prompt, c_sample, ada_w, ada_b, ln_g, ln_b,
              w_in0, w_out0, rel_bias, conv_w, conv_b, dt_bias, a_log, ssd_d, ssd_norm_g,
              ffn_w_up, ffn_w_down,
              w_in1, s5_lam_re, s5_lam_im, s5_log_step, s5_b_re, s5_b_im, s5_c_re, s5_c_im, s5_d, glu_w,
              router_w, router_b, moe_w_up, moe_w_down):
    weights = (ada_w, ada_b, ln_g, ln_b,
               w_in0, w_out0, rel_bias, conv_w, conv_b, dt_bias, a_log, ssd_d, ssd_norm_g,
               ffn_w_up, ffn_w_down,
               w_in1, s5_lam_re, s5_lam_im, s5_log_step, s5_b_re, s5_b_im, s5_c_re, s5_c_im, s5_d, glu_w,
               router_w, router_b, moe_w_up, moe_w_down)
    y_prompt, k_p, v_p, conv_p, ssd_p, s5re_p, s5im_p = run_trunk(
        x_prompt, c_prompt, None, None, None, None, None, None, *weights)
    y_sample, k_s, v_s, conv_s, ssd_s, s5re_s, s5im_s = run_trunk(
        x_sample, c_sample, cache_attn_k, cache_attn_v, state_ssd_conv, state_ssd, state_s5_re, state_s5_im,
        *weights)
    return (y_prompt, y_sample, k_p, v_p, conv_p, ssd_p, s5re_p, s5im_p, k_s, v_s, conv_s, ssd_s, s5re_s, s5im_s)
```

```python
import os
import numpy as np
import concourse.bass as bass
import concourse.mybir as mybir
from concourse.bass_utils import run_bass_kernel_spmd

F32 = mybir.dt.float32
BF16 = mybir.dt.bfloat16
AF = mybir.ActivationFunctionType
ALU = mybir.AluOpType
AX = mybir.AxisListType

NCORES = 8
D = 2048
KC = 16
NP = 2048
NH = 512
NS = 16
T = NP + NS
R_P = NH
R_S = NH + NP + 16
R_GAP = R_S - 3
TT = R_S + NS
TPAD = 2080
IN0W = 5648
ALPHA = 4.0 ** 0.25
LN_EPS = 1e-5
RMS_EPS = 1e-5
NEG = -1e30
SAME_SYNC = True
STOP = int(os.environ.get("MK_STOP", "99"))


CH_ROWS = 16384
WUNITS = ([(f"ada_w{a}", (D, 3 * D)) for a in range(4)] + [("glu_w", (D, 2 * D)), ("w_out0", (D, D)), ("w_in1", (D, D))]
          + [(f"moe_up{e}", (D, 5632)) for e in range(8)] + [(f"moe_dn{e}", (2816, D)) for e in range(8)]
          + [("ffn_g", (D, 5632)), ("ffn_u", (D, 5632)), ("ffn_dn", (5632, D)), ("w_in0", (D, IN0W))]
          + [(n_, (128, 64, 128)) for n_ in ("s5bre", "s5bim", "s5cre", "s5cim")] + [("tblp", (128, 16, 640))])


def _place():
    used = []
    out = {}
    for name, shp in WUNITS:
        n = 1
        for d_ in shp:
            n *= d_
        rows = n // 2048
        assert rows * 2048 == n
        for c in range(len(used) + 1):
            if c == len(used):
                used.append(0)
            if used[c] + rows <= CH_ROWS:
                out[name] = (c, used[c], shp)
                used[c] += rows
                break
    return out, len(used)


WPLACE, NCHUNK = _place()


class Res:
    __slots__ = ("w", "r")

    def __init__(self):
        self.w = {}
        self.r = {}


class TL:
    def __init__(self, h):
        self.h = h
        self.res = Res()

    def __getitem__(self, idx):
        return self.h[idx]


class Prog:
    def __init__(self):
        self.nc = bass.Bass("TRN2", target_bir_lowering=False)
        nc = self.nc
        self.engs = {"pe": nc.tensor, "act": nc.scalar, "dve": nc.vector, "pool": nc.gpsimd, "sp": nc.sync}
        self.cnt = {}
        self.sem = {}
        self.nsem = 0
        for e in ("pe", "act", "dve", "pool"):
            self._new_epoch(e)
        self.known = {e: {} for e in self.engs}
        self.slots = {}
        for q, n in (("sp", 12), ("pool", 12), ("act", 6)):
            self.slots[q] = [[nc.alloc_semaphore(name=f"d{q}{i}"), 0] for i in range(n)]
        self.slot_i = {q: 0 for q in self.slots}
        self.dres = {}
        self.ninst = 0

    def _new_epoch(self, e):
        self.nsem += 1
        self.sem[e] = self.nc.alloc_semaphore(name=f"c{e}{self.nsem}")
        self.cnt[e] = 0

    def R(self, *key):
        r = self.dres.get(key)
        if r is None:
            r = self.dres[key] = Res()
        return r

    def _wait(self, e, ev):
        sem, val = ev
        k = self.known[e]
        if k.get(sem.num, 0) >= val:
            return
        self.engs[e].wait_ge(sem, val)
        k[sem.num] = val

    @staticmethod
    def _res(x):
        return x.res if isinstance(x, TL) else x

    def _deps(self, e, Rd, Wr):
        own = self.sem.get(e)
        evs = {}
        for x in Rd:
            for s, ev in self._res(x).w.items():
                if s not in evs or evs[s][1] < ev[1]:
                    evs[s] = ev
        for x in Wr:
            rr = self._res(x)
            for dd in (rr.w, rr.r):
                for s, ev in dd.items():
                    if s not in evs or evs[s][1] < ev[1]:
                        evs[s] = ev
        for s, ev in evs.items():
            if own is not None and s == own.num:
                if e == "pe" or not SAME_SYNC:
                    continue
            self._wait(e, ev)

    def _mark(self, ev, Rd, Wr):
        s = ev[0].num
        for x in Rd:
            self._res(x).r[s] = ev
        for x in Wr:
            rr = self._res(x)
            rr.w = {s: ev}
            rr.r = {}

    def op(self, e, fn, Rd=(), Wr=()):
        self._deps(e, Rd, Wr)
        if self.cnt[e] >= 30000:
            self._new_epoch(e)
        inst = fn()
        self.cnt[e] += 1
        inst.then_inc(self.sem[e], 1)
        self._mark((self.sem[e], self.cnt[e]), Rd, Wr)
        self.ninst += 1

    def mm(self, out, lhsT, rhs, start, stop, Rd=(), Wr=()):
        self.op("pe", lambda: self.nc.tensor.matmul(out, lhsT=lhsT, rhs=rhs, start=start, stop=stop), Rd, Wr)

    def dma(self, q, out, in_, Rd=(), Wr=(), **kw):
        self._deps(q, Rd, Wr)
        sl = self.slots[q]
        i = self.slot_i[q]
        self.slot_i[q] = (i + 1) % len(sl)
        s = sl[i]
        if s[1] > 0:
            self._wait(q, (s[0], s[1]))
        s[1] += 16
        self.engs[q].dma_start(out=out, in_=in_, **kw).then_inc(s[0], 16)
        self._mark((s[0], s[1]), Rd, Wr)
        self.ninst += 1

    def barrier(self):
        evs = []
        for e in ("pe", "act", "dve", "pool"):
            if self.cnt[e] > 0:
                evs.append((self.sem[e], self.cnt[e]))
        for q, sl in self.slots.items():
            for s in sl:
                if s[1] > 0:
                    evs.append((s[0], s[1]))
        for e in self.engs:
            own = self.sem.get(e)
            for ev in evs:
                if own is not None and ev[0].num == own.num:
                    continue
                self._wait(e, ev)

    def finish(self):
        for q, sl in self.slots.items():
            for s in sl:
                if s[1] > 0:
                    self._wait("sp", (s[0], s[1]))
        for e in ("pe", "act", "dve", "pool"):
            if self.cnt[e] > 0:
                self._wait("sp", (self.sem[e], self.cnt[e]))


def bc(ap, axis, shape):
    return ap.unsqueeze(axis).to_broadcast(shape)


def build():
    P = Prog()
    nc = P.nc
    V, A, G, PE = nc.vector, nc.scalar, nc.gpsimd, nc.tensor

    def din(name, shape, dt=F32):
        return nc.dram_tensor(name, list(shape), dt, kind="ExternalInput").ap()

    def dout(name, shape, dt=F32):
        return nc.dram_tensor(name, list(shape), dt, kind="ExternalOutput").ap()

    def dscr(name, shape, dt=F32):
        return nc.dram_tensor(name, list(shape), dt).ap()

    xp = din("xp", [NP, D]); xh = din("xh", [NH, D]); xs = din("xs", [NS, D])
    cpr = din("cp", [16, 128]); csm = din("cs", [16, 128])
    ck = din("ck", [512, 1024]); cv = din("cv", [512, 1024])
    sconv = din("sconv", [3, 1536]); sssd = din("sssd", [1024, 128])
    s5re0 = din("s5re0", [64, 128]); s5im0 = din("s5im0", [64, 128])
    ada_b = din("ada_b", [4 * 48, 128])
    ln_g = din("ln_g", [64, 128]); ln_b = din("ln_b", [64, 128])
    tbls = din("tbls", [16, 16, 528])
    conv_w = din("conv_w", [4, 1536]); conv_b = din("conv_b", [1, 1536])
    dt_bias = din("dt_bias", [1, 16]); a_log = din("a_log", [1, 16]); ssd_d = din("ssd_d", [1, 16])
    ssd_ng = din("ssd_ng", [1, 1024])
    lamre = din("lamre", [64, 128]); lamim = din("lamim", [64, 128]); lstep = din("lstep", [64, 2])
    s5d = din("s5d", [16, 128])
    router_w = din("router_w", [D, 8]); router_b = din("router_b", [1, 8])
    wsh = din("wsh", [NCHUNK, CH_ROWS // NCORES, 2048])
    wi_c = [nc.dram_tensor(f"wi{c}", [CH_ROWS // NCORES, 2048], F32) for c in range(NCHUNK)]
    wf_c = [nc.dram_tensor(f"wf{c}", [CH_ROWS, 2048], F32) for c in range(NCHUNK)]

    def wv(name):
        c, r0, shp = WPLACE[name]
        n = 1
        for d_ in shp:
            n *= d_
        flat = wf_c[c].ap()[r0:r0 + n // 2048, :].rearrange("r c -> (r c)")
        if len(shp) == 2:
            return flat.rearrange("(k n) -> k n", n=shp[1])
        return flat.rearrange("(a k n) -> a k n", a=shp[0], n=shp[2])

    ada_w = [wv(f"ada_w{a}") for a in range(4)]
    w_in0 = wv("w_in0"); w_out0 = wv("w_out0")
    tblp = wv("tblp")
    ffn_g = wv("ffn_g"); ffn_u = wv("ffn_u"); ffn_dn = wv("ffn_dn")
    w_in1 = wv("w_in1"); glu_w = wv("glu_w")
    s5bre = wv("s5bre"); s5bim = wv("s5bim"); s5cre = wv("s5cre"); s5cim = wv("s5cim")
    moe_up = [wv(f"moe_up{e}") for e in range(8)]; moe_dn = [wv(f"moe_dn{e}") for e in range(8)]
    ident_d = din("ident", [128, 128]); triu_d = din("triu", [128, 128]); mneg_d = din("mneg", [128, 128])
    flag_d = din("flag", [128, 1]); pen_d = din("pen", [128, 1]); oneh_d = din("oneh", [128, 8])
    sel_d = din("sel", [8, 1024]); segm_d = din("segm", [128, 512])

    o_y = dout("o_y", [NP, D]); o_ys = dout("o_ys", [NS, D])
    o_k = dout("o_k", [512, 1024]); o_v = dout("o_v", [512, 1024])
    o_ks = dout("o_ks", [NS, 1024]); o_vs = dout("o_vs", [NS, 1024])
    o_conv = dout("o_conv", [3, 1536]); o_convs = dout("o_convs", [3, 1536])
    o_ssd = dout("o_ssd", [1024, 128]); o_ssds = dout("o_ssds", [1024, 128])
    o_s5re = dout("o_s5re", [64, 128]); o_s5im = dout("o_s5im", [64, 128])
    o_s5res = dout("o_s5res", [64, 128]); o_s5ims = dout("o_s5ims", [64, 128])

    xT = [dscr(f"xT{i}", [D, T]) for i in range(2)]
    xTh = dscr("xTh", [D, NH])
    qT = dscr("qT", [1024, TPAD], BF16); kT = dscr("kT", [1024, TT], BF16)
    vS = dscr("vS", [TT, 1024]); kS32 = dscr("kS32", [TT, 1024]); zS = dscr("zS", [T, 1024], BF16)
    xbcS = dscr("xbcS", [TT, 1536]); dtS = dscr("dtS", [T, 16])
    yT = dscr("yT", [D, TPAD], BF16)
    ylocS = dscr("ylocS", [NP, 1024])
    uT = dscr("uT", [D, TPAD], BF16)
    xch_in = nc.dram_tensor("xch_in", [128, 1040], F32)
    xch_out = nc.dram_tensor("xch_out", [NCORES * 128, 1040], F32)
    xch5_in = nc.dram_tensor("xch5_in", [128, 128], F32)
    xch5_out = nc.dram_tensor("xch5_out", [NCORES * 128, 128], F32)

    def fm(ap):
        return ap.rearrange("(kc p) t -> p kc t", p=128)

    def sb(name, shape, dt=F32):
        return TL(nc.alloc_sbuf_tensor("sb_" + name, list(shape), dt))

    PS = [TL(nc.alloc_psum_tensor(f"ps{i}", [128, 512], F32)) for i in range(8)]
    ident = sb("ident", [128, 128]); identb = sb("identb", [128, 128], BF16)
    ones = sb("ones", [128, 128]); onesb = sb("onesb", [128, 128], BF16)
    flag = sb("flag", [128, 1]); pen = sb("pen", [128, 1]); oneh = sb("oneh", [128, 8])
    mod = sb("mod", [128, 4, 2, 48])
    sc1 = sb("sc1", [128, 4, 2, 16]); g1 = sb("g1", [128, 4, 2, 16])
    lng = sb("lng", [128, 64]); lnb = sb("lnb", [128, 64])
    epsc = sb("epsc", [128, 1]); onec = sb("onec", [128, 1])

    blocks = [(128 * b, 128, 0) for b in range(16)] + [(NP, NS, 1)]
    groups = [blocks[0:4], blocks[4:8], blocks[8:12], blocks[12:17]]

    def trow(blk):
        c0, n, s = blk
        return R_S if s else R_P + c0

    ccs = nc.alloc_semaphore(name="ccsem")
    for c in range(NCHUNK):
        P.dma("pool", wi_c[c].ap(), wsh[c], Wr=[P.R("wi", c)])
    for c in range(NCHUNK):
        P._deps("pool", [P.R("wi", c)], [])
        nc.gpsimd.collective_compute("AllGather", ALU.bypass, replica_groups=[list(range(NCORES))],
                                     ins=[wi_c[c].ap().opt()], outs=[wf_c[c].ap().opt()]).then_inc(ccs)
    for e_ in P.engs:
        P.engs[e_].wait_ge(ccs, NCHUNK)
    P.dma("sp", ident[:], ident_d, Wr=[ident])
    P.dma("sp", flag[:], flag_d, Wr=[flag]); P.dma("sp", pen[:], pen_d, Wr=[pen]); P.dma("sp", oneh[:], oneh_d, Wr=[oneh])
    P.op("dve", lambda: V.tensor_copy(out=identb[:], in_=ident[:]), [ident], [identb])
    P.op("dve", lambda: V.memset(ones[:], 1.0), [], [ones])
    P.op("dve", lambda: V.memset(onesb[:], 1.0), [], [onesb])
    P.op("dve", lambda: V.memset(epsc[:], LN_EPS), [], [epsc])
    P.op("dve", lambda: V.memset(onec[:], 1.0), [], [onec])

    def transpose_to(dst_ap, dstT, src_ap, srcT, npart_in, nfree_in, ps, eng="dve"):
        P.op("pe", lambda: PE.transpose(out=ps[0:nfree_in, 0:npart_in], in_=src_ap, identity=ident[0:npart_in, 0:npart_in]),
             [srcT, ident], [ps])
        if eng == "dve":
            P.op("dve", lambda: V.tensor_copy(out=dst_ap, in_=ps[0:nfree_in, 0:npart_in]), [ps], [dstT])
        else:
            P.op("act", lambda: A.copy(out=dst_ap, in_=ps[0:nfree_in, 0:npart_in]), [ps], [dstT])

    tmpA = sb("tmpA", [64, 128]); tmpB = sb("tmpB", [64, 128])
    P.dma("sp", tmpA[:], ln_g, Wr=[tmpA]); P.dma("sp", tmpB[:], ln_b, Wr=[tmpB])
    transpose_to(lng[:], lng, tmpA[:], tmpA, 64, 128, PS[0])
    transpose_to(lnb[:], lnb, tmpB[:], tmpB, 64, 128, PS[1])
    sT = sb("sT", [128, 16, 2], BF16)
    cc = sb("cc", [16, 2, 128]); ccT = sb("ccT", [128, 2, 16])
    P.dma("sp", cc[:, 0, :], cpr, Wr=[cc]); P.dma("sp", cc[:, 1, :], csm, Wr=[cc])
    for s in range(2):
        transpose_to(ccT[:, s, :], ccT, cc[:, s, :], cc, 16, 128, PS[s])
    for s in range(2):
        P.op("act", lambda s=s: A.activation(out=sT[:, :, s], in_=ccT[:, s, :], func=AF.Silu), [ccT], [sT])
    adab = sb("adab", [128, 192]); adabr = sb("adabr", [96, 2, 128])
    P.dma("sp", adabr[:], ada_b.rearrange("(h p) f -> p h f", p=96), Wr=[adabr])
    for hh in range(2):
        transpose_to(adab[:, hh * 96:(hh + 1) * 96], adab, adabr[:, hh, :], adabr, 96, 128, PS[2 + hh])
    from contextlib import ExitStack

    def mk_dense(es, tag, with_x=True):
        def sbp(name, shape, dt=F32):
            return TL(es.enter_context(nc.sbuf_tensor(f"{tag}_{name}", list(shape), dt)))
        xin_ = [sbp(f"xin{i}", [128, D]) for i in range(2 if with_x else 0)]
        xst_ = [sbp(f"xst{i}", [128, 16, 128]) for i in range(2 if with_x else 0)]
        return (sbp("xg", [128, 16, 528]), sbp("hT", [128, 16, 528], BF16), [sbp(f"wts{i}", [128, 16, 512], BF16) for i in range(2)],
                xin_, xst_, [sbp(f"stbf{i}", [128, 528], BF16) for i in range(2)], [sbp(f"stf{i}", [128, 512]) for i in range(2)])

    esA = ExitStack()
    xg, hT, wts, xin, xst, st_bf, st_f32 = mk_dense(esA, "dA")
    wi = [0]

    def wtile():
        t = wts[wi[0] % 2]
        wi[0] += 1
        return t

    if STOP >= 0:
        psm = PS[4]
        for a in range(4):
            for ct in range(12):
                wt = wtile()
                P.dma("pool", wt[:], fm(ada_w[a])[:, :, ct * 512:(ct + 1) * 512], Wr=[wt])
                for mc in range(4):
                    ch = ct * 4 + mc
                    for kc in range(KC):
                        P.mm(psm[:, ch * 2:ch * 2 + 2], wt[:, kc, mc * 128:(mc + 1) * 128], sT[:, kc, :], kc == 0, kc == KC - 1,
                             [wt, sT], [psm])
            for s in range(2):
                P.op("dve", lambda a=a, s=s: V.tensor_tensor(out=mod[:, a, s, :],
                                                              in0=psm[:, 0:96].rearrange("p (c s) -> p s c", s=2)[:, s, :],
                                                              in1=adab[:, a * 48:(a + 1) * 48], op=ALU.add), [psm, adab], [mod])
        P.op("dve", lambda: V.tensor_scalar(out=sc1[:], in0=mod[:, :, :, 16:32], scalar1=1.0, scalar2=None, op0=ALU.add), [mod], [sc1])
        P.op("dve", lambda: V.tensor_scalar(out=g1[:], in0=mod[:, :, :, 32:48], scalar1=1.0, scalar2=None, op0=ALU.add), [mod], [g1])


    def stage_xT(src_rows_ap, ntok, dst_ap, dres, i):
        xi = xin[i % 2]; xo = xst[i % 2]
        P.dma("sp", xi[0:ntok, :], src_rows_ap, Wr=[xi])
        for q4 in range(4):
            ps = PS[(i * 4 + q4) % 4]
            for j in range(4):
                kc = q4 * 4 + j
                P.op("pe", lambda kc=kc, j=j, ps=ps: PE.transpose(out=ps[:, j * 128:j * 128 + ntok], in_=xi[0:ntok, kc * 128:(kc + 1) * 128],
                                                                identity=ident[0:ntok, 0:ntok]), [xi, ident], [ps])
            src = ps[:, :].rearrange("p (j t) -> p j t", j=4)[:, :, 0:ntok]
            if q4 % 2 == 0:
                P.op("dve", lambda q4=q4, src=src: V.tensor_copy(out=xo[:, q4 * 4:(q4 + 1) * 4, 0:ntok], in_=src), [ps], [xo])
            else:
                P.op("act", lambda q4=q4, src=src: A.copy(out=xo[:, q4 * 4:(q4 + 1) * 4, 0:ntok], in_=src), [ps], [xo])
        P.dma("sp", dst_ap, xo[:, :, 0:ntok], Rd=[xo], Wr=[dres])

    i = 0
    for hb in range(4):
        stage_xT(xh[hb * 128:(hb + 1) * 128, :], 128, fm(xTh)[:, :, hb * 128:(hb + 1) * 128], P.R("xTh", hb), i); i += 1
    for bi, (c0, n, s) in enumerate(blocks):
        src = xs[:, :] if s else xp[c0:c0 + n, :]
        stage_xT(src, n, fm(xT[0])[:, :, c0:c0 + n], P.R("xT0", bi), i); i += 1

    def segs(grp):
        out = []
        off = 0
        for (c0, n, s) in grp:
            if out and out[-1][2] == s:
                out[-1] = (out[-1][0], out[-1][1] + n, s)
            else:
                out.append((off, n, s))
            off += n
        return out, off


    def load_group_h(src_fm, c0, n, resl, a, sg):
        P.dma("sp", xg[:, :, 0:n], src_fm[:, :, c0:c0 + n], Rd=resl, Wr=[xg])
        for kc in range(KC):
            for (o, m, s) in sg:
                P.op("act", lambda kc=kc, o=o, m=m, s=s: A.activation(out=hT[:, kc, o:o + m], in_=xg[:, kc, o:o + m], func=AF.Identity,
                                                                      scale=sc1[:, a, s, kc:kc + 1], bias=mod[:, a, s, kc:kc + 1]),
                     [xg, sc1, mod], [hT])

    sti = [0]

    def inproj_group(grp, halo, gi):
        if halo:
            n = NH; sg = [(0, NH, 0)]
            P.dma("sp", xg[:, :, 0:n], fm(xTh)[:, :, :], Rd=[P.R("xTh", b) for b in range(4)], Wr=[xg])
            for kc in range(KC):
                P.op("act", lambda: A.activation(out=hT[:, kc, 0:n], in_=xg[:, kc, 0:n], func=AF.Identity,
                                                 scale=sc1[:, 0, 0, kc:kc + 1], bias=mod[:, 0, 0, kc:kc + 1]), [xg, sc1, mod], [hT])
            blks = [(128 * b, 128, 0) for b in range(4)]
            trows = [128 * b for b in range(4)]
        else:
            sg, n = segs(grp)
            load_group_h(fm(xT[0]), grp[0][0], n, [P.R("xT0", blocks.index(b)) for b in grp], 0, sg)
            blks = grp
            trows = [trow(b) for b in grp]
        jobs = []
        if not halo:
            jobs += [("fm", 0, qT, 0), ("fm", 512, qT, 512)]
        jobs += [("fm", 1024, kT, 0), ("fm", 1536, kT, 512)]
        if not halo:
            jobs += [("tm", 1024, kS32, 0), ("tm", 1536, kS32, 512)]
        jobs += [("tm", 2048, vS, 0), ("tm", 2560, vS, 512)]
        if not halo:
            jobs += [("tmz", 3072, zS, 0), ("tmz", 3584, zS, 512)]
        jobs += [("tm", 4096, xbcS, 0), ("tm", 4608, xbcS, 512), ("tm", 5120, xbcS, 1024)]
        if not halo:
            jobs += [("dt", IN0W - 512, dtS, 0)]
        for (kind, wc0, dstT, dc0) in jobs:
            wt = wtile()
            P.dma("pool", wt[:, :, :], fm(w_in0)[:, :, wc0:wc0 + 512], Wr=[wt])
            if kind == "fm":
                isq = dstT is qT
                for mc in range(4):
                    row0 = dc0 + mc * 128
                    for (so, sn, ss) in sg:
                        ps = PS[sti[0] % 2]
                        stg = st_bf[sti[0] % 2]; sti[0] += 1
                        for kc in range(KC):
                            P.mm(ps[:, 0:sn], wt[:, kc, mc * 128:(mc + 1) * 128], hT[:, kc, so:so + sn], kc == 0, kc == KC - 1, [wt, hT], [ps])
                        if halo:
                            P.op("act", lambda: A.activation(out=stg[:, 0:sn], in_=ps[:, 0:sn], func=AF.Identity, scale=flag[:, 0:1]), [ps, flag], [stg])
                            dcol = so
                        else:
                            P.op("act", lambda: A.copy(out=stg[:, 0:sn], in_=ps[:, 0:sn]), [ps], [stg])
                            if isq:
                                dcol = (grp[0][0] + so) if ss == 0 else NP
                            else:
                                dcol = (R_P + grp[0][0] + so) if ss == 0 else R_S
                        P.dma("sp", dstT[row0:row0 + 128, dcol:dcol + sn], stg[:, 0:sn], Rd=[stg], Wr=[P.R("fmst", id(dstT), row0, dcol)])
            else:
                bo = 0
                for bi, (c0b, nb, sb_) in enumerate(blks):
                    ps = PS[2 + sti[0] % 2]
                    r0 = trows[bi]
                    zr = NP if sb_ else c0b
                    for kc in range(KC):
                        P.mm(ps[0:nb, 0:512], hT[:, kc, bo:bo + nb], wt[:, kc, 0:512], kc == 0, kc == KC - 1, [wt, hT], [ps])
                    if kind == "tmz":
                        stg = st_bf[sti[0] % 2]; sti[0] += 1
                        P.op("act", lambda: A.copy(out=stg[0:nb, 0:512], in_=ps[0:nb, :]), [ps], [stg])
                        P.dma("sp", zS[zr:zr + nb, dc0:dc0 + 512], stg[0:nb, 0:512], Rd=[stg], Wr=[P.R("zS", zr, dc0)])
                    else:
                        stg = st_f32[sti[0] % 2]; sti[0] += 1
                        if halo:
                            P.op("dve", lambda: V.tensor_scalar(out=stg[0:nb, :], in0=ps[0:nb, :], scalar1=flag[0:nb, 0:1], scalar2=None, op0=ALU.mult),
                                 [ps, flag], [stg])
                        else:
                            P.op("dve", lambda: V.tensor_copy(out=stg[0:nb, :], in_=ps[0:nb, :]), [ps], [stg])
                        if kind == "dt":
                            P.dma("sp", dtS[zr:zr + nb, :], stg[0:nb, 496:512], Rd=[stg], Wr=[P.R("dtS", zr)])
                        else:
                            P.dma("sp", dstT[r0:r0 + nb, dc0:dc0 + 512], stg[0:nb, :], Rd=[stg], Wr=[P.R("tmst", id(dstT), r0)])
                    bo += nb

    if STOP >= 1:
        DBG = os.environ.get("MK_DBG", "")
        if DBG != "nohalo":
            inproj_group(None, True, -1)
        for gi, grp in enumerate(groups):
            if DBG == "halo" or (DBG == "g0" and gi > 0) or (DBG in ("g3", "g3ns", "g3nl") and gi != 3) or (DBG == "g01" and gi > 1):
                continue
            inproj_group(grp, False, gi)
        P.barrier()
        P.dma("sp", o_conv[:, :], xbcS[R_P + NP - 3:R_P + NP, :], Wr=[P.R("oconv")])
        P.dma("sp", o_convs[:, :], xbcS[R_S + NS - 3:R_S + NS, :], Wr=[P.R("oconvs")])
        P.dma("sp", xbcS[R_GAP:R_GAP + 3, :], sconv[:, :], Wr=[P.R("xbcS", "gap")])
        for b4 in range(4):
            P.dma("sp", o_k[b4 * 128:(b4 + 1) * 128, :], kS32[R_P + NP - 512 + b4 * 128:R_P + NP - 512 + (b4 + 1) * 128, :], Wr=[P.R("ok", b4)])
            P.dma("sp", o_v[b4 * 128:(b4 + 1) * 128, :], vS[R_P + NP - 512 + b4 * 128:R_P + NP - 512 + (b4 + 1) * 128, :], Wr=[P.R("ov", b4)])
        P.dma("sp", o_ks[:, :], kS32[R_S:R_S + NS, :], Wr=[P.R("oks")])
        P.dma("sp", o_vs[:, :], vS[R_S:R_S + NS, :], Wr=[P.R("ovs")])

    P.barrier()
    esA.close()
    psb = TL(PS[7].h)
    psb.res = PS[7].res
    PSB = PS[7][:, :].bitcast(BF16)

    def attention(es):
        def sbp(name, shape, dt=F32):
            return TL(es.enter_context(nc.sbuf_tensor("p2_" + name, list(shape), dt)))
        tbl = sbp("tbl", [128, 16, 640]); tbs = sbp("tbs", [16, 16, 528])
        P.dma("sp", tbl[:], tblp, Wr=[tbl]); P.dma("sp", tbs[:], tbls, Wr=[tbs])
        P.op("pool", lambda: G.memset(tbl[0:64, :, 576:640], NEG), [], [tbl])
        P.op("pool", lambda: G.memset(tbl[64:128, :, 0:64], NEG), [], [tbl])
        kTw = sbp("kTw", [128, 8, 640], BF16); Vw = sbp("Vw", [128, 5, 1024], BF16)
        qTb = sbp("qTb", [128, 8, 128], BF16)
        s_sb = sbp("s_sb", [128, 640]); p_sb = sbp("p_sb", [128, 640], BF16); pT = sbp("pT", [128, 640], BF16)
        att = sbp("att", [128, 1024], BF16); attT = sbp("attT", [128, 8, 128], BF16)
        nmx = sbp("nmx", [128, 1]); rs = sbp("rs", [128, 1]); rrs = sbp("rrs", [128, 1])

        def heads(nq, kA, nA, kB, nB, pv, tb, pen_cols, qt, kres, vres):
            ntot = nA + nB
            for h in range(16):
                fc, r0 = h // 2, (h % 2) * 64
                P.mm(PS[0][0:nq, 0:nA], qt[r0:r0 + 64, fc, 0:nq], kA(fc, r0), True, True, [qt] + kres, [PS[0]])
                P.mm(PS[1][0:nq, 0:nB], qt[r0:r0 + 64, fc, 0:nq], kB(fc, r0), True, True, [qt] + kres, [PS[1]])
                P.op("dve", lambda: V.scalar_tensor_tensor(out=s_sb[0:nq, 0:nA], in0=PS[0][0:nq, 0:nA], scalar=0.125, in1=tb[0:nq, h, 0:nA],
                                                           op0=ALU.mult, op1=ALU.add), [PS[0], tb], [s_sb])
                P.op("dve", lambda: V.scalar_tensor_tensor(out=s_sb[0:nq, nA:ntot], in0=PS[1][0:nq, 0:nB], scalar=0.125, in1=tb[0:nq, h, nA:ntot],
                                                           op0=ALU.mult, op1=ALU.add), [PS[1], tb], [s_sb])
                if pen_cols:
                    P.op("dve", lambda: V.tensor_scalar(out=s_sb[0:nq, 0:pen_cols], in0=s_sb[0:nq, 0:pen_cols], scalar1=pen[0:nq, 0:1], scalar2=None,
                                                        op0=ALU.add), [s_sb, pen], [s_sb])
                P.op("dve", lambda: V.tensor_reduce(out=nmx[0:nq, :], in_=s_sb[0:nq, 0:ntot], axis=AX.X, op=ALU.max, negate=True), [s_sb], [nmx])
                P.op("act", lambda: A.activation(out=p_sb[0:nq, 0:ntot], in_=s_sb[0:nq, 0:ntot], func=AF.Exp, bias=nmx[0:nq, 0:1], scale=1.0,
                                                 accum_out=rs[0:nq, 0:1]), [s_sb, nmx], [p_sb, rs])
                P.op("dve", lambda: V.reciprocal(out=rrs[0:nq, :], in_=rs[0:nq, :]), [rs], [rrs])
                for i, (pc, nk, vf) in enumerate(pv):
                    P.op("pe", lambda: PE.transpose(out=PSB[0:nk, i * 128:i * 128 + nq], in_=p_sb[0:nq, pc:pc + nk], identity=identb[0:nq, 0:nq]),
                         [p_sb, identb], [PS[7]])
                    P.op("act", lambda: A.copy(out=pT[0:nk, i * 128:i * 128 + nq], in_=PSB[0:nk, i * 128:i * 128 + nq]), [PS[7]], [pT])
                for i, (pc, nk, vf) in enumerate(pv):
                    P.mm(PS[2][0:nq, 0:64], pT[0:nk, i * 128:i * 128 + nq], vf(h), i == 0, i == len(pv) - 1, [pT] + vres, [PS[2]])
                P.op("act", lambda: A.activation(out=att[0:nq, h * 64:(h + 1) * 64], in_=PS[2][0:nq, 0:64], func=AF.Identity, scale=rrs[0:nq, 0:1]),
                     [PS[2], rrs], [att])
            for fc in range(8):
                P.op("pe", lambda: PE.transpose(out=PSB[:, fc * 128:fc * 128 + nq], in_=att[0:nq, fc * 128:(fc + 1) * 128], identity=identb[0:nq, 0:nq]),
                     [att, identb], [PS[7]])
            P.op("dve", lambda: V.tensor_copy(out=attT[:, :, 0:nq], in_=PSB[:, :].rearrange("p (f t) -> p f t", f=8)[:, :, 0:nq]), [PS[7]], [attT])

        for j in range(16):
            P.dma("sp", kTw[:], kT.rearrange("(fc p) t -> p fc t", p=128)[:, :, 128 * j:128 * j + 640], Wr=[kTw])
            P.dma("pool", Vw[:], vS[128 * j:128 * j + 640, :].rearrange("(kb p) f -> p kb f", p=128), Wr=[Vw])
            P.dma("sp", qTb[:], qT.rearrange("(fc p) t -> p fc t", p=128)[:, :, 128 * j:128 * j + 128], Wr=[qTb])
            pv = [(kb * 128, 128, (lambda h, kb=kb: Vw[:, kb, h * 64:(h + 1) * 64])) for kb in range(5)]
            heads(128, lambda fc, r0: kTw[r0:r0 + 64, fc, 0:512], 512, lambda fc, r0: kTw[r0:r0 + 64, fc, 512:640], 128, pv, tbl,
                  max(0, 512 - 128 * j), qTb, [kTw], [Vw])
            P.dma("sp", fm(yT)[:, 0:8, 128 * j:128 * j + 128], attT[:, :, :], Rd=[attT], Wr=[P.R("yTa", j)])
        kTc = sbp("kTc", [128, 8, 512], BF16); kTn = sbp("kTn", [128, 8, 16], BF16)
        Vn = sbp("Vn", [16, 1024], BF16); ckb = sbp("ckb", [128, 1024])
        for kb in range(4):
            P.dma("sp", ckb[:], ck[kb * 128:(kb + 1) * 128, :], Wr=[ckb])
            for half in range(2):
                ps = PS[3 + half]
                for f4 in range(4):
                    fc = half * 4 + f4
                    P.op("pe", lambda: PE.transpose(out=ps[:, f4 * 128:(f4 + 1) * 128], in_=ckb[:, fc * 128:(fc + 1) * 128], identity=ident[:, :]),
                         [ckb, ident], [ps])
                P.op("dve", lambda: V.tensor_copy(out=kTc[:, half * 4:(half + 1) * 4, kb * 128:(kb + 1) * 128],
                                                   in_=ps[:, :].rearrange("p (f t) -> p f t", f=4)), [ps], [kTc])
        P.dma("pool", Vw[:, 0:4, :], cv.rearrange("(kb p) f -> p kb f", p=128), Wr=[Vw])
        P.dma("sp", kTn[:], kT.rearrange("(fc p) t -> p fc t", p=128)[:, :, R_S:R_S + NS], Wr=[kTn])
        P.dma("pool", Vn[:], vS[R_S:R_S + NS, :], Wr=[Vn])
        P.dma("sp", qTb[:, :, 0:NS], qT.rearrange("(fc p) t -> p fc t", p=128)[:, :, NP:NP + NS], Wr=[qTb])
        pv = [(kb * 128, 128, (lambda h, kb=kb: Vw[:, kb, h * 64:(h + 1) * 64])) for kb in range(4)]
        pv.append((512, 16, lambda h: Vn[0:16, h * 64:(h + 1) * 64]))
        heads(NS, lambda fc, r0: kTc[r0:r0 + 64, fc, 0:512], 512, lambda fc, r0: kTn[r0:r0 + 64, fc, 0:16], 16, pv, tbs, 0, qTb, [kTc, kTn], [Vw, Vn])
        P.dma("sp", fm(yT)[:, 0:8, NP:NP + NS], attT[:, :, 0:NS], Rd=[attT], Wr=[P.R("yTa", 16)])

    if STOP >= 2:
        with ExitStack() as es:
            attention(es)
            P.barrier()

    def ssd(es):
        def sbp(name, shape, dt=F32):
            return TL(es.enter_context(nc.sbuf_tensor("p3_" + name, list(shape), dt)))
        cwb = sbp("cwb", [128, 4, 1536]); cbb = sbp("cbb", [128, 1, 1536])
        dtb = sbp("dtb", [128, 1, 16]); Ab = sbp("Ab", [128, 1, 16]); Dsk = sbp("Dsk", [128, 1, 16]); ngb = sbp("ngb", [128, 1, 1024])
        triU = sbp("triU", [128, 128]); mneg = sbp("mneg", [128, 128])
        P.dma("sp", cwb[:], conv_w.partition_broadcast(128), Wr=[cwb]); P.dma("sp", cbb[:], conv_b.partition_broadcast(128), Wr=[cbb])
        P.dma("sp", dtb[:], dt_bias.partition_broadcast(128), Wr=[dtb]); P.dma("sp", Ab[:], a_log.partition_broadcast(128), Wr=[Ab])
        P.dma("sp", Dsk[:], ssd_d.partition_broadcast(128), Wr=[Dsk]); P.dma("sp", ngb[:], ssd_ng.partition_broadcast(128), Wr=[ngb])
        P.dma("sp", triU[:], triu_d, Wr=[triU]); P.dma("sp", mneg[:], mneg_d, Wr=[mneg])
        P.op("act", lambda: A.activation(out=Ab[:], in_=Ab[:], func=AF.Exp), [Ab], [Ab])
        P.op("dve", lambda: V.tensor_scalar(out=Ab[:], in0=Ab[:], scalar1=-1.0, scalar2=None, op0=ALU.mult), [Ab], [Ab])
        xs4 = sbp("xs4", [128, 4, 1536]); acc = sbp("acc", [128, 1536]); xc = sbp("xc", [128, 1536]); tmpc = sbp("tmpc", [128, 1536])
        dtr = sbp("dtr", [128, 16]); dtt = sbp("dtt", [128, 16]); av = sbp("av", [128, 16]); aU = sbp("aU", [128, 16, 128])
        acs = sbp("acs", [128, 16]); nacs = sbp("nacs", [128, 16]); eacs = sbp("eacs", [128, 16]); t16 = sbp("t16", [128, 16])
        arg = sbp("arg", [128, 4, 128]); dec = sbp("dec", [128, 4, 128]); ST = sbp("ST", [128, 16, 128], BF16)
        BT = sbp("BT", [128, 2, 128], BF16); CTall = sbp("CTall", [128, 17, 2, 128], BF16); Bb = sbp("Bb", [128, 2, 128], BF16)
        xdt = sbp("xdt", [128, 1024], BF16); xdtw = sbp("xdtw", [128, 1024], BF16)
        yl = sbp("yl", [128, 1024]); t1 = sbp("t1", [128, 512]); t2 = sbp("t2", [128, 512])
        toend = sbp("toend", [128, 16]); cdec = sbp("cdec", [128, 16]); Dprev = sbp("Dprev", [128, 16]); eag = sbp("eag", [128, 17, 16])
        h32 = sbp("h32", [128, 1024]); hb = sbp("hb", [128, 1024], BF16)
        zt = sbp("zt", [128, 1024], BF16); sz = sbp("sz", [128, 1024]); yg = sbp("yg", [128, 1024]); yn = sbp("yn", [128, 1024], BF16)
        ynT = sbp("ynT", [128, 8, 128], BF16); ss = sbp("ss", [128, 1]); rstd = sbp("rstd", [128, 1])
        rmsc = sbp("rmsc", [128, 1])
        P.op("dve", lambda: V.memset(rmsc[:], RMS_EPS), [], [rmsc])
        xst3 = [sbp("xst3a", [128, 8, 128]), sbp("xst3b", [128, 8, 128])]

        def finish_blk(n, zrow, col0):
            P.dma("sp", zt[0:n, :], zS[zrow:zrow + n, :], Wr=[zt])
            P.op("act", lambda: A.activation(out=sz[0:n, :], in_=zt[0:n, :], func=AF.Silu), [zt], [sz])
            P.op("dve", lambda: V.tensor_tensor(out=yg[0:n, :], in0=yl[0:n, :], in1=sz[0:n, :], op=ALU.mult), [yl, sz], [yg])
            P.op("act", lambda: A.activation(out=sz[0:n, :], in_=yg[0:n, :], func=AF.Square, accum_out=ss[0:n, 0:1]), [yg], [sz, ss])
            P.op("act", lambda: A.activation(out=rstd[0:n, :], in_=ss[0:n, :], func=AF.Sqrt, bias=rmsc[0:n, 0:1], scale=1.0 / 1024.0), [ss, rmsc], [rstd])
            P.op("dve", lambda: V.reciprocal(out=rstd[0:n, :], in_=rstd[0:n, :]), [rstd], [rstd])
            P.op("dve", lambda: V.scalar_tensor_tensor(out=yn[0:n, :], in0=yg[0:n, :], scalar=rstd[0:n, 0:1], in1=ngb[0:n, 0, :], op0=ALU.mult, op1=ALU.mult),
                 [yg, rstd, ngb], [yn])
            for fc in range(8):
                P.op("pe", lambda: PE.transpose(out=PSB[:, fc * 128:fc * 128 + n], in_=yn[0:n, fc * 128:(fc + 1) * 128], identity=identb[0:n, 0:n]),
                     [yn, identb], [PS[7]])
            P.op("act", lambda: A.copy(out=ynT[:, :, 0:n], in_=PSB[:, :].rearrange("p (f t) -> p f t", f=8)[:, :, 0:n]), [PS[7]], [ynT])
            P.dma("sp", fm(yT)[:, 8:16, col0:col0 + n], ynT[:, :, 0:n], Rd=[ynT], Wr=[P.R("yTs", col0)])

        def block(blk, r0, n, zrow):
            for tap in range(4):
                P.dma("sp", xs4[0:n, tap, :], xbcS[r0 + tap - 3:r0 + tap - 3 + n, :], Wr=[xs4])
            P.dma("sp", dtr[0:n, :], dtS[zrow:zrow + n, :], Wr=[dtr])
            P.op("pool", lambda: G.tensor_tensor(out=acc[0:n, :], in0=xs4[0:n, 0, :], in1=cwb[0:n, 0, :], op=ALU.mult), [xs4, cwb], [acc])
            for tap in range(1, 4):
                P.op("pool", lambda: G.tensor_tensor(out=tmpc[0:n, :], in0=xs4[0:n, tap, :], in1=cwb[0:n, tap, :], op=ALU.mult), [xs4, cwb], [tmpc])
                P.op("dve", lambda: V.tensor_tensor(out=acc[0:n, :], in0=acc[0:n, :], in1=tmpc[0:n, :], op=ALU.add), [acc, tmpc], [acc])
            P.op("dve", lambda: V.tensor_tensor(out=acc[0:n, :], in0=acc[0:n, :], in1=cbb[0:n, 0, :], op=ALU.add), [acc, cbb], [acc])
            P.op("act", lambda: A.activation(out=xc[0:n, :], in_=acc[0:n, :], func=AF.Silu), [acc], [xc])
            P.op("dve", lambda: V.tensor_tensor(out=dtt[0:n, :], in0=dtr[0:n, :], in1=dtb[0:n, 0, :], op=ALU.add), [dtr, dtb], [dtt])
            P.op("act", lambda: A.activation(out=dtt[0:n, :], in_=dtt[0:n, :], func=AF.Exp), [dtt], [dtt])
            P.op("act", lambda: A.activation(out=dtt[0:n, :], in_=dtt[0:n, :], func=AF.Ln, bias=onec[0:n, 0:1], scale=1.0), [dtt, onec], [dtt])
            P.op("dve", lambda: V.tensor_tensor(out=av[0:n, :], in0=dtt[0:n, :], in1=Ab[0:n, 0, :], op=ALU.mult), [dtt, Ab], [av])
            P.mm(PS[0][0:n, 0:16], triU[0:n, 0:n], av[0:n, :], True, True, [triU, av], [PS[0]])
            P.op("dve", lambda: V.tensor_copy(out=acs[0:n, :], in_=PS[0][0:n, 0:16]), [PS[0]], [acs])
            P.op("dve", lambda: V.tensor_scalar(out=nacs[0:n, :], in0=acs[0:n, :], scalar1=-1.0, scalar2=None, op0=ALU.mult), [acs], [nacs])
            P.op("act", lambda: A.activation(out=eacs[0:n, :], in_=acs[0:n, :], func=AF.Exp), [acs], [eacs])
            P.op("dve", lambda: V.tensor_tensor(out=t16[0:n, :], in0=acs[0:n, :], in1=Dprev[0:n, :], op=ALU.add), [acs, Dprev], [t16])
            P.op("act", lambda: A.activation(out=eag[0:n, blk, :], in_=t16[0:n, :], func=AF.Exp), [t16], [eag])
            P.op("dve", lambda: V.tensor_tensor(out=aU[0:n, :, 0:n], in0=av[0:n, :].unsqueeze(2).to_broadcast([n, 16, n]),
                                                 in1=triU[0:n, 0:n].unsqueeze(1).to_broadcast([n, 16, n]), op=ALU.mult), [av, triU], [aU])
            for q in range(4):
                P.mm(PS[1 + q][:, 0:4 * n], ones[0:n, :], aU[0:n, 4 * q:4 * q + 4, 0:n], True, True, [ones, aU], [PS[1 + q]])
            for g in range(2):
                P.op("pe", lambda: PE.transpose(out=PS[5][:, g * 128:g * 128 + n], in_=xc[0:n, 1024 + 128 * g:1152 + 128 * g], identity=ident[0:n, 0:n]),
                     [xc, ident], [PS[5]])
                P.op("pe", lambda: PE.transpose(out=PS[5][:, 256 + g * 128:256 + g * 128 + n], in_=xc[0:n, 1280 + 128 * g:1408 + 128 * g],
                                                identity=ident[0:n, 0:n]), [xc, ident], [PS[5]])
            for g in range(2):
                P.op("act", lambda: A.copy(out=BT[:, g, 0:n], in_=PS[5][:, g * 128:g * 128 + n]), [PS[5]], [BT])
                P.op("act", lambda: A.copy(out=CTall[:, blk, g, 0:n], in_=PS[5][:, 256 + g * 128:256 + g * 128 + n]), [PS[5]], [CTall])
            P.op("pool", lambda: G.tensor_copy(out=Bb[0:n, :, :], in_=xc[0:n, 1024:1280].rearrange("p (g s) -> p g s", g=2)), [xc], [Bb])
            for g in range(2):
                P.mm(PS[6][0:n, g * 128:g * 128 + n], BT[:, g, 0:n], CTall[:, blk, g, 0:n], True, True, [BT, CTall], [PS[6]])
            P.op("pool", lambda: G.tensor_tensor(out=xdt[0:n, :].rearrange("p (h d) -> p h d", h=16), in0=xc[0:n, 0:1024].rearrange("p (h d) -> p h d", h=16),
                                                  in1=dtt[0:n, :].unsqueeze(2).to_broadcast([n, 16, 64]), op=ALU.mult), [xc, dtt], [xdt])
            for q in range(4):
                g = q // 2
                Rv = PS[1 + q][:, 0:4 * n].rearrange("p (h i) -> p h i", h=4)
                P.op("dve", lambda: V.tensor_tensor(out=arg[0:n, :, 0:n], in0=Rv[0:n, :, :], in1=mneg[0:n, 0:n].unsqueeze(1).to_broadcast([n, 4, n]),
                                                     op=ALU.add), [PS[1 + q], mneg], [arg])
                for hh in range(4):
                    h = 4 * q + hh
                    P.op("act", lambda: A.activation(out=dec[0:n, hh, 0:n], in_=arg[0:n, hh, 0:n], func=AF.Exp, bias=nacs[0:n, h:h + 1], scale=1.0),
                         [arg, nacs], [dec])
                P.op("dve", lambda: V.tensor_tensor(out=ST[0:n, 4 * q:4 * q + 4, 0:n], in0=PS[6][0:n, g * 128:g * 128 + n].unsqueeze(1).to_broadcast([n, 4, n]),
                                                     in1=dec[0:n, :, 0:n], op=ALU.mult), [PS[6], dec], [ST])
                P.op("dve", lambda: V.tensor_tensor(out=t16[0:n, 4 * q:4 * q + 4], in0=Rv[0:n, :, n - 1], in1=acs[0:n, 4 * q:4 * q + 4], op=ALU.subtract),
                     [PS[1 + q], acs], [t16])
                P.op("act", lambda: A.activation(out=cdec[:, 4 * q:4 * q + 4], in_=Rv[:, :, n - 1], func=AF.Exp), [PS[1 + q]], [cdec])
                P.op("dve", lambda: V.tensor_tensor(out=Dprev[:, 4 * q:4 * q + 4], in0=Dprev[:, 4 * q:4 * q + 4], in1=Rv[:, :, n - 1], op=ALU.add),
                     [PS[1 + q], Dprev], [Dprev])
            P.op("act", lambda: A.activation(out=toend[0:n, :], in_=t16[0:n, :], func=AF.Exp), [t16], [toend])
            P.op("pool", lambda: G.tensor_tensor(out=xdtw[0:n, :].rearrange("p (h d) -> p h d", h=16), in0=xdt[0:n, :].rearrange("p (h d) -> p h d", h=16),
                                                  in1=toend[0:n, :].unsqueeze(2).to_broadcast([n, 16, 64]), op=ALU.mult), [xdt, toend], [xdtw])
            for g in range(2):
                for hh in range(8):
                    h = 8 * g + hh
                    P.mm(PS[1 + g][0:n, hh * 64:(hh + 1) * 64], ST[0:n, h, 0:n], xdt[0:n, h * 64:(h + 1) * 64], True, True, [ST, xdt], [PS[1 + g]])
                P.mm(PS[3 + g][0:n, 0:512], CTall[:, blk, g, 0:n], hb[:, g * 512:(g + 1) * 512], True, True, [CTall, hb], [PS[3 + g]])
                P.op("dve", lambda: V.tensor_tensor(out=t1[0:n, :].rearrange("p (h d) -> p h d", h=8), in0=PS[3 + g][0:n, :].rearrange("p (h d) -> p h d", h=8),
                                                     in1=eacs[0:n, 8 * g:8 * g + 8].unsqueeze(2).to_broadcast([n, 8, 64]), op=ALU.mult), [PS[3 + g], eacs], [t1])
                P.op("dve", lambda: V.tensor_tensor(out=t1[0:n, :], in0=t1[0:n, :], in1=PS[1 + g][0:n, :], op=ALU.add), [t1, PS[1 + g]], [t1])
                P.op("pool", lambda: G.tensor_tensor(out=t2[0:n, :].rearrange("p (h d) -> p h d", h=8),
                                                      in0=xc[0:n, g * 512:(g + 1) * 512].rearrange("p (h d) -> p h d", h=8),
                                                      in1=Dsk[0:n, 0, 8 * g:8 * g + 8].unsqueeze(2).to_broadcast([n, 8, 64]), op=ALU.mult), [xc, Dsk], [t2])
                P.op("dve", lambda: V.tensor_tensor(out=yl[0:n, g * 512:(g + 1) * 512], in0=t1[0:n, :], in1=t2[0:n, :], op=ALU.add), [t1, t2], [yl])
                pst = PS[5] if g == 0 else PS[0]
                P.mm(pst[:, 0:512], Bb[0:n, g, :], xdtw[0:n, g * 512:(g + 1) * 512], True, True, [Bb, xdtw], [pst])
                P.op("dve", lambda: V.tensor_tensor(out=h32[:, g * 512:(g + 1) * 512].rearrange("p (h d) -> p h d", h=8),
                                                     in0=h32[:, g * 512:(g + 1) * 512].rearrange("p (h d) -> p h d", h=8),
                                                     in1=cdec[:, 8 * g:8 * g + 8].unsqueeze(2).to_broadcast([128, 8, 64]), op=ALU.mult), [h32, cdec], [h32])
                P.op("dve", lambda: V.tensor_tensor(out=h32[:, g * 512:(g + 1) * 512], in0=h32[:, g * 512:(g + 1) * 512], in1=pst[:, 0:512], op=ALU.add),
                     [h32, pst], [h32])
                P.op("act", lambda: A.copy(out=hb[:, g * 512:(g + 1) * 512], in_=h32[:, g * 512:(g + 1) * 512]), [h32], [hb])

        def state_out(dst):
            for c in range(8):
                transpose_to(xst3[0][:, c, :], xst3[0], h32[:, c * 128:(c + 1) * 128], h32, 128, 128, PS[c % 4])
            P.dma("sp", dst.rearrange("(c p) n -> p c n", p=128), xst3[0][:, 0:8, :], Rd=[xst3[0]], Wr=[P.R("ossd", id(dst))])

        P.op("dve", lambda: V.memset(h32[:], 0.0), [], [h32]); P.op("dve", lambda: V.memset(hb[:], 0.0), [], [hb])
        P.op("dve", lambda: V.memset(Dprev[:], 0.0), [], [Dprev])
        for b in range(16):
            block(b, R_P + 128 * b, 128, 128 * b)
            P.dma("sp", ylocS[128 * b:128 * (b + 1), :], yl[:, :], Rd=[yl], Wr=[P.R("yloc", b)])
        P.dma("sp", xch_in.ap()[:, 0:1024], h32[:, :], Rd=[h32], Wr=[P.R("xch")])
        P.dma("sp", xch_in.ap()[:, 1024:1040], Dprev[:, :], Rd=[Dprev], Wr=[P.R("xch")])
        P._deps("pool", [P.R("xch")], [])
        ccs2 = nc.alloc_semaphore(name="ccsem2")
        nc.gpsimd.collective_compute("AllGather", ALU.bypass, replica_groups=[list(range(NCORES))],
                                     ins=[xch_in.ap().opt()], outs=[xch_out.ap().opt()]).then_inc(ccs2)
        for e_ in P.engs:
            P.engs[e_].wait_ge(ccs2, 1)
        SA = sbp("SA", [128, 8, 1040]); Hk = sbp("Hk", [128, 1024]); Hm = sbp("Hm", [128, 1024]); Hb = sbp("Hb", [128, 1024], BF16)
        eD = sbp("eD", [128, 16])
        P.dma("sp", SA[:], xch_out.ap().rearrange("(r p) f -> p r f", p=128), Wr=[SA])
        P.op("dve", lambda: V.memset(Hk[:], 0.0), [], [Hk]); P.op("dve", lambda: V.memset(Hm[:], 0.0), [], [Hm])
        for k in range(NCORES):
            P.op("dve", lambda: V.scalar_tensor_tensor(out=Hm[:], in0=Hk[:], scalar=oneh[:, k:k + 1], in1=Hm[:], op0=ALU.mult, op1=ALU.add), [Hk, oneh, Hm], [Hm])
            P.op("act", lambda: A.activation(out=eD[:], in_=SA[:, k, 1024:1040], func=AF.Exp), [SA], [eD])
            P.op("dve", lambda: V.tensor_tensor(out=Hk[:].rearrange("p (h d) -> p h d", h=16), in0=Hk[:].rearrange("p (h d) -> p h d", h=16),
                                                 in1=eD[:].unsqueeze(2).to_broadcast([128, 16, 64]), op=ALU.mult), [Hk, eD], [Hk])
            P.op("dve", lambda: V.tensor_tensor(out=Hk[:], in0=Hk[:], in1=SA[:, k, 0:1024], op=ALU.add), [Hk, SA], [Hk])
        P.op("act", lambda: A.copy(out=Hb[:], in_=Hm[:]), [Hm], [Hb])
        P.op("dve", lambda: V.tensor_copy(out=h32[:], in_=Hk[:]), [Hk], [h32])
        state_out(o_ssd)
        for b in range(16):
            P.dma("sp", yl[:, :], ylocS[128 * b:128 * (b + 1), :], Wr=[yl])
            for g in range(2):
                P.mm(PS[g][:, 0:512], CTall[:, b, g, :], Hb[:, g * 512:(g + 1) * 512], True, True, [CTall, Hb], [PS[g]])
                P.op("dve", lambda: V.tensor_tensor(out=t1[:, :].rearrange("p (h d) -> p h d", h=8), in0=PS[g][:, :].rearrange("p (h d) -> p h d", h=8),
                                                     in1=eag[:, b, 8 * g:8 * g + 8].unsqueeze(2).to_broadcast([128, 8, 64]), op=ALU.mult), [PS[g], eag], [t1])
                P.op("dve", lambda: V.tensor_tensor(out=yl[:, g * 512:(g + 1) * 512], in0=yl[:, g * 512:(g + 1) * 512], in1=t1[:, :], op=ALU.add), [yl, t1], [yl])
            finish_blk(128, 128 * b, 128 * b)
        P.dma("sp", xst3[1][:, 0:8, :], sssd.rearrange("(c p) n -> p c n", p=128), Wr=[xst3[1]])
        for c in range(8):
            transpose_to(h32[:, c * 128:(c + 1) * 128], h32, xst3[1][:, c, :], xst3[1], 128, 128, PS[c % 4])
        P.op("act", lambda: A.copy(out=hb[:], in_=h32[:]), [h32], [hb])
        P.op("dve", lambda: V.memset(Dprev[:], 0.0), [], [Dprev])
        block(16, R_S, NS, NP)
        finish_blk(NS, NP, NP)
        state_out(o_ssds)

    if STOP >= 3:
        with ExitStack() as es:
            ssd(es)
            P.barrier()


    def wtile_view(kch, cw):
        t = wtile()
        v = t.h[:, :, :].rearrange("p a b -> p (a b)")[:, 0:kch * cw].rearrange("p (k c) -> p k c", c=cw)
        return t, v

    pidx = [0]

    def proj_fm(waps, kch, ncols, src, sg, evac, cw):
        for c0 in range(0, ncols, cw):
            tiles = []
            for wap in waps:
                t, v = wtile_view(kch, cw)
                P.dma("pool", v, wap.rearrange("(kc p) c -> p kc c", p=128)[:, :, c0:c0 + cw], Wr=[t])
                tiles.append((t, v))
            for mcl in range(cw // 128):
                mc = c0 // 128 + mcl
                base = (pidx[0] % 2) * 4
                pidx[0] += 1
                res = []
                for wi_, (t, v) in enumerate(tiles):
                    lst = []
                    for si, (o, m, s) in enumerate(sg):
                        ps = PS[base + wi_ * 2 + si]
                        for kc in range(kch):
                            P.mm(ps[:, 0:m], v[:, kc, mcl * 128:(mcl + 1) * 128], src[:, kc, o:o + m], kc == 0, kc == kch - 1, [t, src], [ps])
                        lst.append((ps, o, m, s))
                    res.append(lst)
                evac(mc, res)

    def dense_bufs(es, tag):
        def sbp(name, shape, dt=F32):
            return TL(es.enter_context(nc.sbuf_tensor(f"{tag}_{name}", list(shape), dt)))
        return sbp

    def ln_group(sbx, a, n, sg, store):
        rT, xg_, sq2, mean, rstd_, tmpn = sbx["rT"], sbx["xg"], sbx["sq"], sbx["mean"], sbx["rstd"], sbx["tmpn"]
        for kc in range(KC):
            sq = sq2[kc % 2]
            P.op("act", lambda: A.activation(out=sq[:, 0:n], in_=rT[:, kc, 0:n], func=AF.Square), [rT], [sq])
            for si, (o, m, s) in enumerate(sg):
                P.mm(PS[4 + si][:, 0:m], ones[:, :], rT[:, kc, o:o + m], kc == 0, kc == KC - 1, [ones, rT], [PS[4 + si]])
                P.mm(PS[6 + si][:, 0:m], ones[:, :], sq[:, o:o + m], kc == 0, kc == KC - 1, [ones, sq], [PS[6 + si]])
        for si, (o, m, s) in enumerate(sg):
            P.op("act", lambda: A.mul(out=mean[:, o:o + m], in_=PS[4 + si][:, 0:m], mul=1.0 / D), [PS[4 + si]], [mean])
            P.op("dve", lambda: V.tensor_tensor(out=tmpn[:, o:o + m], in0=mean[:, o:o + m], in1=mean[:, o:o + m], op=ALU.mult), [mean], [tmpn])
            P.op("dve", lambda: V.scalar_tensor_tensor(out=tmpn[:, o:o + m], in0=PS[6 + si][:, 0:m], scalar=1.0 / D, in1=tmpn[:, o:o + m],
                                                       op0=ALU.mult, op1=ALU.subtract), [PS[6 + si], tmpn], [tmpn])
            P.op("act", lambda: A.activation(out=rstd_[:, o:o + m], in_=tmpn[:, o:o + m], func=AF.Sqrt, bias=epsc[:, 0:1], scale=1.0), [tmpn, epsc], [rstd_])
            P.op("dve", lambda: V.reciprocal(out=rstd_[:, o:o + m], in_=rstd_[:, o:o + m]), [rstd_], [rstd_])
        for kc in range(KC):
            P.op("dve", lambda: V.tensor_tensor(out=rT[:, kc, 0:n], in0=rT[:, kc, 0:n], in1=mean[:, 0:n], op=ALU.subtract), [rT, mean], [rT])
            P.op("dve", lambda: V.tensor_tensor(out=rT[:, kc, 0:n], in0=rT[:, kc, 0:n], in1=rstd_[:, 0:n], op=ALU.mult), [rT, rstd_], [rT])
            P.op("act", lambda: A.activation(out=xg_[:, kc, 0:n], in_=rT[:, kc, 0:n], func=AF.Identity, scale=lng[:, a * 16 + kc:a * 16 + kc + 1],
                                             bias=lnb[:, a * 16 + kc:a * 16 + kc + 1]), [rT, lng, lnb], [xg_])
        store()

    def mk_sbx(sbp):
        return dict(rT=sbp("rT", [128, 16, 528]), xg=xg, sq=[sbp("sq0", [128, 528]), sbp("sq1", [128, 528])], mean=sbp("mean", [128, 528]),
                    rstd=sbp("rstd", [128, 528]), tmpn=sbp("tmpn", [128, 528]))

    def load_x(src_xT, grp, a_mod, want_h=True, h32=None):
        sg, n = segs(grp)
        c0 = grp[0][0]
        P.dma("sp", xg[:, :, 0:n], fm(src_xT)[:, :, c0:c0 + n], Wr=[xg])
        if want_h:
            for kc in range(KC):
                for (o, m, s) in sg:
                    P.op("act", lambda: A.activation(out=hT[:, kc, o:o + m], in_=xg[:, kc, o:o + m], func=AF.Identity,
                                                     scale=sc1[:, a_mod, s, kc:kc + 1], bias=mod[:, a_mod, s, kc:kc + 1]), [xg, sc1, mod], [hT])
                    if h32 is not None:
                        P.op("dve", lambda: V.tensor_scalar(out=h32[:, kc, o:o + m], in0=xg[:, kc, o:o + m], scalar1=sc1[:, a_mod, s, kc:kc + 1],
                                                            scalar2=mod[:, a_mod, s, kc:kc + 1], op0=ALU.mult, op1=ALU.add), [xg, sc1, mod], [h32])
        P.op("pool", lambda: G.tensor_scalar(out=xg[:, :, 0:n], in0=xg[:, :, 0:n], scalar1=ALPHA, scalar2=None, op0=ALU.mult), [xg], [xg])
        return sg, n, c0

    def combine_evac(sbx, a):
        rT = sbx["rT"]

        def ev(mc, res):
            for (ps, o, m, s) in res[0]:
                P.op("dve", lambda: V.scalar_tensor_tensor(out=rT[:, mc, o:o + m], in0=ps[:, 0:m], scalar=g1[:, a, s, mc:mc + 1], in1=xg[:, mc, o:o + m],
                                                           op0=ALU.mult, op1=ALU.add), [ps, g1, xg], [rT])
        return ev

    def store_xT(dst_xT, c0, n):
        def st():
            P.dma("sp", fm(dst_xT)[:, :, c0:c0 + n], xg[:, :, 0:n], Rd=[xg], Wr=[P.R("xTst", id(dst_xT), c0)])
        return st

    esB = ExitStack()
    if STOP >= 4:
        xg, hT, wts, xin, xst, st_bf, st_f32 = mk_dense(esB, "dB", with_x=False)
    if STOP >= 4:
        with ExitStack() as es:
            sbx = mk_sbx(dense_bufs(es, "p4"))
            for grp in groups:
                sg, n, c0 = load_x(xT[0], grp, 0, want_h=False)
                P.dma("sp", hT[:, :, 0:n], fm(yT)[:, :, c0:c0 + n], Wr=[hT])
                proj_fm([w_out0], 16, D, hT, sg, combine_evac(sbx, 0), 512)
                ln_group(sbx, 0, n, sg, store_xT(xT[1], c0, n))
            P.barrier()

    if STOP >= 5:
        with ExitStack() as es:
            sbp = dense_bufs(es, "p5")
            sbx = mk_sbx(sbp)
            hid = sbp("hid", [128, 44, 528], BF16); sgt = [sbp("sgt0", [128, 528]), sbp("sgt1", [128, 528])]
            for grp in groups:
                sg, n, c0 = load_x(xT[1], grp, 1)

                def ev_up(mc, res):
                    t_ = sgt[mc % 2]
                    for (psg, o, m, s), (psu, _, _, _) in zip(res[0], res[1]):
                        P.op("act", lambda: A.activation(out=t_[:, o:o + m], in_=psg[:, 0:m], func=AF.Silu), [psg], [t_])
                        P.op("dve", lambda: V.tensor_tensor(out=hid[:, mc, o:o + m], in0=t_[:, o:o + m], in1=psu[:, 0:m], op=ALU.mult), [t_, psu], [hid])
                proj_fm([ffn_g, ffn_u], 16, 5632, hT, sg, ev_up, 512)
                proj_fm([ffn_dn], 44, D, hid, sg, combine_evac(sbx, 1), 128)
                ln_group(sbx, 1, n, sg, store_xT(xT[0], c0, n))
            P.barrier()

    if STOP >= 6:
        for grp in groups:
            sg, n, c0 = load_x(xT[0], grp, 2)

            def ev_u(mc, res):
                for (ps, o, m, s) in res[0]:
                    stg = st_bf[sti[0] % 2]; sti[0] += 1
                    P.op("act", lambda: A.copy(out=stg[:, 0:m], in_=ps[:, 0:m]), [ps], [stg])
                    P.dma("sp", uT[mc * 128:(mc + 1) * 128, c0 + o:c0 + o + m], stg[:, 0:m], Rd=[stg], Wr=[P.R("uT", mc, c0 + o)])
            proj_fm([w_in1], 16, D, hT, sg, ev_u, 512)
        P.barrier()

    esB.close()

    L5 = 64
    PI = float(np.pi)

    def s5(es):
        sbp = dense_bufs(es, "p7")
        lr = sbp("lr", [128, 64]); li = sbp("li", [128, 64]); stp = sbp("stp", [128, 64])
        t64 = [sbp(f"t64_{i}", [128, 64]) for i in range(8)]
        ti = TL(es.enter_context(nc.sbuf_tensor("p7_ti", [128, 64], mybir.dt.int32)))
        abr = sbp("abr", [128, 64]); abi = sbp("abi", [128, 64]); fre = sbp("fre", [128, 64]); fim = sbp("fim", [128, 64])
        ldt = sbp("ldt", [64, 128]); ls2 = sbp("ls2", [64, 2])
        for src, dst in ((lamre, lr), (lamim, li)):
            P.dma("sp", ldt[:], src, Wr=[ldt])
            transpose_to(dst[:], dst, ldt[:], ldt, 64, 128, PS[0])
        P.dma("sp", ls2[:], lstep, Wr=[ls2])
        for gi in range(2):
            P.op("dve", lambda: V.tensor_copy(out=ldt[:, gi * 64:(gi + 1) * 64], in_=ls2[:, gi:gi + 1].to_broadcast([64, 64])), [ls2], [ldt])
        transpose_to(stp[:], stp, ldt[:], ldt, 64, 128, PS[0])
        P.op("act", lambda: A.activation(out=stp[:], in_=stp[:], func=AF.Exp), [stp], [stp])
        mag, ang, sn, cs = t64[0], t64[1], t64[2], t64[3]
        P.op("dve", lambda: V.tensor_tensor(out=mag[:], in0=lr[:], in1=stp[:], op=ALU.mult), [lr, stp], [mag])
        P.op("act", lambda: A.activation(out=mag[:], in_=mag[:], func=AF.Exp), [mag], [mag])
        P.op("dve", lambda: V.tensor_tensor(out=ang[:], in0=li[:], in1=stp[:], op=ALU.mult), [li, stp], [ang])

        def sin_of(dst, shift):
            x, kf, msk = t64[4], t64[5], t64[6]
            P.op("dve", lambda: V.tensor_scalar(out=x[:], in0=ang[:], scalar1=shift, scalar2=None, op0=ALU.add), [ang], [x])
            P.op("dve", lambda: V.tensor_scalar(out=kf[:], in0=x[:], scalar1=1.0 / (2 * PI), scalar2=None, op0=ALU.mult), [x], [kf])
            P.op("dve", lambda: V.tensor_copy(out=ti[:], in_=kf[:]), [kf], [ti])
            P.op("dve", lambda: V.tensor_copy(out=kf[:], in_=ti[:]), [ti], [kf])
            P.op("dve", lambda: V.scalar_tensor_tensor(out=x[:], in0=kf[:], scalar=-2 * PI, in1=x[:], op0=ALU.mult, op1=ALU.add), [kf, x], [x])
            P.op("dve", lambda: V.tensor_single_scalar(out=msk[:], in_=x[:], scalar=PI, op=ALU.is_gt), [x], [msk])
            P.op("dve", lambda: V.scalar_tensor_tensor(out=x[:], in0=msk[:], scalar=-2 * PI, in1=x[:], op0=ALU.mult, op1=ALU.add), [msk, x], [x])
            P.op("dve", lambda: V.tensor_single_scalar(out=msk[:], in_=x[:], scalar=-PI, op=ALU.is_lt), [x], [msk])
            P.op("dve", lambda: V.scalar_tensor_tensor(out=x[:], in0=msk[:], scalar=2 * PI, in1=x[:], op0=ALU.mult, op1=ALU.add), [msk, x], [x])
            P.op("act", lambda: A.activation(out=dst[:], in_=x[:], func=AF.Sin), [x], [dst])
        sin_of(sn, 0.0)
        sin_of(cs, PI / 2)
        P.op("dve", lambda: V.tensor_tensor(out=abr[:], in0=mag[:], in1=cs[:], op=ALU.mult), [mag, cs], [abr])
        P.op("dve", lambda: V.tensor_tensor(out=abi[:], in0=mag[:], in1=sn[:], op=ALU.mult), [mag, sn], [abi])
        am1, den, nr, ni, tq = t64[1], t64[2], t64[3], t64[4], t64[5]
        P.op("dve", lambda: V.tensor_scalar(out=am1[:], in0=abr[:], scalar1=-1.0, scalar2=None, op0=ALU.add), [abr], [am1])
        P.op("dve", lambda: V.tensor_tensor(out=den[:], in0=lr[:], in1=lr[:], op=ALU.mult), [lr], [den])
        P.op("dve", lambda: V.tensor_tensor(out=tq[:], in0=li[:], in1=li[:], op=ALU.mult), [li], [tq])
        P.op("dve", lambda: V.tensor_tensor(out=den[:], in0=den[:], in1=tq[:], op=ALU.add), [den, tq], [den])
        P.op("dve", lambda: V.reciprocal(out=den[:], in_=den[:]), [den], [den])
        P.op("dve", lambda: V.tensor_tensor(out=nr[:], in0=am1[:], in1=lr[:], op=ALU.mult), [am1, lr], [nr])
        P.op("dve", lambda: V.tensor_tensor(out=tq[:], in0=abi[:], in1=li[:], op=ALU.mult), [abi, li], [tq])
        P.op("dve", lambda: V.tensor_tensor(out=nr[:], in0=nr[:], in1=tq[:], op=ALU.add), [nr, tq], [nr])
        P.op("dve", lambda: V.tensor_tensor(out=ni[:], in0=abi[:], in1=lr[:], op=ALU.mult), [abi, lr], [ni])
        P.op("dve", lambda: V.tensor_tensor(out=tq[:], in0=am1[:], in1=li[:], op=ALU.mult), [am1, li], [tq])
        P.op("dve", lambda: V.tensor_tensor(out=ni[:], in0=ni[:], in1=tq[:], op=ALU.subtract), [ni, tq], [ni])
        P.op("dve", lambda: V.tensor_tensor(out=fre[:], in0=nr[:], in1=den[:], op=ALU.mult), [nr, den], [fre])
        P.op("dve", lambda: V.tensor_tensor(out=fim[:], in0=ni[:], in1=den[:], op=ALU.mult), [ni, den], [fim])
        fsc = [dscr("fsc_re", [64, 128]), dscr("fsc_im", [64, 128])]
        for src, dd in ((fre, fsc[0]), (fim, fsc[1])):
            transpose_to(ldt[:], ldt, src[:], src, 128, 64, PS[0])
            P.dma("sp", dd, ldt[:], Rd=[ldt], Wr=[P.R("fsc", id(dd))])
        BBre = sbp("BBre", [128, 64, 128], BF16); BBim = sbp("BBim", [128, 64, 128], BF16)
        Cre = sbp("Cre", [128, 64, 128], BF16); Cim = sbp("Cim", [128, 64, 128], BF16)
        with ExitStack() as es2:
            def sb2(name, shape, dt=F32):
                return TL(es2.enter_context(nc.sbuf_tensor("p7b_" + name, list(shape), dt)))
            Fr = sb2("Fr", [128, 16, 128]); Fi = sb2("Fi", [128, 16, 128]); bzr = sb2("bzr", [128, 16, 128]); bzi = sb2("bzi", [128, 16, 128])
            u1 = sb2("u1", [128, 16, 128]); u2 = sb2("u2", [128, 16, 128])
            for qc in range(4):
                qs = slice(qc * 16, (qc + 1) * 16)
                P.dma("sp", Fr[:], fsc[0][qc * 16:(qc + 1) * 16, :].partition_broadcast(128), Rd=[P.R("fsc", id(fsc[0]))], Wr=[Fr])
                P.dma("sp", Fi[:], fsc[1][qc * 16:(qc + 1) * 16, :].partition_broadcast(128), Rd=[P.R("fsc", id(fsc[1]))], Wr=[Fi])
                P.dma("sp", bzr[:], s5bre[:, qs, :], Wr=[bzr]); P.dma("sp", bzi[:], s5bim[:, qs, :], Wr=[bzi])
                P.op("dve", lambda: V.tensor_tensor(out=u1[:], in0=Fr[:], in1=bzr[:], op=ALU.mult), [Fr, bzr], [u1])
                P.op("pool", lambda: G.tensor_tensor(out=u2[:], in0=Fi[:], in1=bzi[:], op=ALU.mult), [Fi, bzi], [u2])
                P.op("dve", lambda: V.tensor_tensor(out=BBre[:, qs, :], in0=u1[:], in1=u2[:], op=ALU.subtract), [u1, u2], [BBre])
                P.op("dve", lambda: V.tensor_tensor(out=u1[:], in0=Fr[:], in1=bzi[:], op=ALU.mult), [Fr, bzi], [u1])
                P.op("pool", lambda: G.tensor_tensor(out=u2[:], in0=Fi[:], in1=bzr[:], op=ALU.mult), [Fi, bzr], [u2])
                P.op("dve", lambda: V.tensor_tensor(out=BBim[:, qs, :], in0=u1[:], in1=u2[:], op=ALU.add), [u1, u2], [BBim])
            P.barrier()
        P.dma("pool", Cre[:], s5cre, Wr=[Cre]); P.dma("pool", Cim[:], s5cim, Wr=[Cim])
        P.op("pool", lambda: G.tensor_scalar(out=Cim[:], in0=Cim[:], scalar1=-1.0, scalar2=None, op0=ALU.mult), [Cim], [Cim])
        dsk = sbp("dsk", [128, 16]); d16 = sbp("d16", [16, 128])
        P.dma("sp", d16[:], s5d, Wr=[d16])
        transpose_to(dsk[:], dsk, d16[:], d16, 16, 128, PS[0])
        pwr = sbp("pwr", [128, 64, L5]); pwi = sbp("pwi", [128, 64, L5]); ipr = sbp("ipr", [128, 64, L5]); ipi = sbp("ipi", [128, 64, L5])
        m2 = t64[0]; iar = t64[1]; iai = t64[2]
        P.op("dve", lambda: V.tensor_tensor(out=m2[:], in0=abr[:], in1=abr[:], op=ALU.mult), [abr], [m2])
        P.op("dve", lambda: V.tensor_tensor(out=tq[:], in0=abi[:], in1=abi[:], op=ALU.mult), [abi], [tq])
        P.op("dve", lambda: V.tensor_tensor(out=m2[:], in0=m2[:], in1=tq[:], op=ALU.add), [m2, tq], [m2])
        P.op("dve", lambda: V.reciprocal(out=m2[:], in_=m2[:]), [m2], [m2])
        P.op("dve", lambda: V.tensor_tensor(out=iar[:], in0=abr[:], in1=m2[:], op=ALU.mult), [abr, m2], [iar])
        P.op("dve", lambda: V.scalar_tensor_tensor(out=iai[:], in0=abi[:], scalar=-1.0, in1=m2[:], op0=ALU.mult, op1=ALU.mult), [abi, m2], [iai])
        tw1 = sbp("tw1", [128, 64, 32]); tw2 = sbp("tw2", [128, 64, 32])

        def build_pow(tr, tim, br, bi_):
            P.op("dve", lambda: V.tensor_copy(out=tr[:, :, 0], in_=br[:]), [br], [tr])
            P.op("dve", lambda: V.tensor_copy(out=tim[:, :, 0], in_=bi_[:]), [bi_], [tim])
            m = 1
            while m < L5:
                cr = tr[:, :, m - 1:m].to_broadcast([128, 64, m]); ci = tim[:, :, m - 1:m].to_broadcast([128, 64, m])
                P.op("dve", lambda: V.tensor_tensor(out=tw1[:, :, 0:m], in0=tr[:, :, 0:m], in1=cr, op=ALU.mult), [tr], [tw1])
                P.op("dve", lambda: V.tensor_tensor(out=tw2[:, :, 0:m], in0=tim[:, :, 0:m], in1=ci, op=ALU.mult), [tim], [tw2])
                P.op("dve", lambda: V.tensor_tensor(out=tr[:, :, m:2 * m], in0=tw1[:, :, 0:m], in1=tw2[:, :, 0:m], op=ALU.subtract), [tw1, tw2], [tr])
                P.op("dve", lambda: V.tensor_tensor(out=tw1[:, :, 0:m], in0=tr[:, :, 0:m], in1=ci, op=ALU.mult), [tr, tim], [tw1])
                P.op("dve", lambda: V.tensor_tensor(out=tw2[:, :, 0:m], in0=tim[:, :, 0:m], in1=cr, op=ALU.mult), [tim, tr], [tw2])
                P.op("dve", lambda: V.tensor_tensor(out=tim[:, :, m:2 * m], in0=tw1[:, :, 0:m], in1=tw2[:, :, 0:m], op=ALU.add), [tw1, tw2], [tim])
                m *= 2
        build_pow(pwr, pwi, abr, abi)
        build_pow(ipr, ipi, iar, iai)
        Ar = sbp("Ar", [128, 64]); Ai = sbp("Ai", [128, 64])
        P.op("dve", lambda: V.tensor_copy(out=Ar[:], in_=pwr[:, :, L5 - 1]), [pwr], [Ar])
        P.op("dve", lambda: V.tensor_copy(out=Ai[:], in_=pwi[:, :, L5 - 1]), [pwi], [Ai])
        for _ in range(5):
            a1, a2 = t64[3], t64[4]
            P.op("dve", lambda: V.tensor_tensor(out=a1[:], in0=Ar[:], in1=Ar[:], op=ALU.mult), [Ar], [a1])
            P.op("dve", lambda: V.tensor_tensor(out=a2[:], in0=Ai[:], in1=Ai[:], op=ALU.mult), [Ai], [a2])
            P.op("dve", lambda: V.tensor_tensor(out=a2[:], in0=a1[:], in1=a2[:], op=ALU.subtract), [a1, a2], [a2])
            P.op("dve", lambda: V.tensor_tensor(out=a1[:], in0=Ar[:], in1=Ai[:], op=ALU.mult), [Ar, Ai], [a1])
            P.op("dve", lambda: V.tensor_scalar(out=Ai[:], in0=a1[:], scalar1=2.0, scalar2=None, op0=ALU.mult), [a1], [Ai])
            P.op("dve", lambda: V.tensor_copy(out=Ar[:], in_=a2[:]), [a2], [Ar])
        segm = sbp("segm", [128, 512])
        P.dma("sp", segm[:], segm_d, Wr=[segm])
        cre = sbp("cre", [128, 64]); cim = sbp("cim", [128, 64])
        uTb = sbp("uTb", [128, 16, L5], BF16); yo = sbp("yo", [128, 16, L5], BF16)
        wre = sbp("wre", [128, 8, L5]); wim = sbp("wim", [128, 8, L5]); tm1 = sbp("tm1", [128, 8, L5]); tm2 = sbp("tm2", [128, 8, L5])
        Wr_ = sbp("Wr", [128, 8, L5]); Wi_ = sbp("Wi", [128, 8, L5]); sr32 = sbp("sr32", [128, 8, L5]); si32 = sbp("si32", [128, 8, L5])
        srb = sbp("srb", [128, 8, L5], BF16); sib = sbp("sib", [128, 8, L5], BF16)
        yv = sbp("yv", [128, L5]); y2 = sbp("y2", [128, L5]); y3 = sbp("y3", [128, L5])
        c1 = sbp("c1", [128, 8]); c2 = sbp("c2", [128, 8])

        def flat(t_):
            return t_[:, :, :].rearrange("p a b -> p (a b)")

        def scan_block(col0, Lc, full):
            P.dma("sp", uTb[:, :, 0:Lc], fm(uT)[:, :, col0:col0 + Lc], Wr=[uTb])
            for qb in range(8):
                qs = slice(8 * qb, 8 * qb + 8)
                for pi_ in range(8):
                    q = 8 * qb + pi_
                    P.mm(PS[0][:, pi_ * L5:pi_ * L5 + Lc], BBre[:, q, :], uTb[:, q // 4, 0:Lc], True, True, [BBre, uTb], [PS[0]])
                    P.mm(PS[1][:, pi_ * L5:pi_ * L5 + Lc], BBim[:, q, :], uTb[:, q // 4, 0:Lc], True, True, [BBim, uTb], [PS[1]])
                bre = PS[0][:, :].rearrange("p (a b) -> p a b", a=8)[:, :, 0:Lc]
                bim = PS[1][:, :].rearrange("p (a b) -> p a b", a=8)[:, :, 0:Lc]
                P.op("dve", lambda: V.tensor_tensor(out=wre[:, :, 0:Lc], in0=bre, in1=ipr[:, qs, 0:Lc], op=ALU.mult), [PS[0], ipr], [wre])
                P.op("dve", lambda: V.tensor_tensor(out=tm1[:, :, 0:Lc], in0=bim, in1=ipi[:, qs, 0:Lc], op=ALU.mult), [PS[1], ipi], [tm1])
                P.op("pool", lambda: G.tensor_tensor(out=wre[:, :, 0:Lc], in0=wre[:, :, 0:Lc], in1=tm1[:, :, 0:Lc], op=ALU.subtract), [wre, tm1], [wre])
                P.op("dve", lambda: V.tensor_tensor(out=wim[:, :, 0:Lc], in0=bre, in1=ipi[:, qs, 0:Lc], op=ALU.mult), [PS[0], ipi], [wim])
                P.op("dve", lambda: V.tensor_tensor(out=tm2[:, :, 0:Lc], in0=bim, in1=ipr[:, qs, 0:Lc], op=ALU.mult), [PS[1], ipr], [tm2])
                P.op("pool", lambda: G.tensor_tensor(out=wim[:, :, 0:Lc], in0=wim[:, :, 0:Lc], in1=tm2[:, :, 0:Lc], op=ALU.add), [wim, tm2], [wim])
                if Lc == L5:
                    P.op("dve", lambda: V.tensor_tensor_scan(out=flat(Wr_), data0=segm[:, :], data1=flat(wre), initial=0.0, op0=ALU.mult, op1=ALU.add),
                         [segm, wre], [Wr_])
                    P.op("dve", lambda: V.tensor_tensor_scan(out=flat(Wi_), data0=segm[:, :], data1=flat(wim), initial=0.0, op0=ALU.mult, op1=ALU.add),
                         [segm, wim], [Wi_])
                else:
                    for pi_ in range(8):
                        P.op("dve", lambda: V.tensor_tensor_scan(out=Wr_[:, pi_, 0:Lc], data0=segm[:, 0:Lc], data1=wre[:, pi_, 0:Lc], initial=0.0,
                                                                 op0=ALU.mult, op1=ALU.add), [segm, wre], [Wr_])
                        P.op("dve", lambda: V.tensor_tensor_scan(out=Wi_[:, pi_, 0:Lc], data0=segm[:, 0:Lc], data1=wim[:, pi_, 0:Lc], initial=0.0,
                                                                 op0=ALU.mult, op1=ALU.add), [segm, wim], [Wi_])
                if not full:
                    P.op("dve", lambda: V.tensor_tensor(out=c1[:], in0=Wr_[:, :, Lc - 1], in1=cre[:, qs], op=ALU.add), [Wr_, cre], [c1])
                    P.op("dve", lambda: V.tensor_tensor(out=c2[:], in0=Wi_[:, :, Lc - 1], in1=cim[:, qs], op=ALU.add), [Wi_, cim], [c2])
                    P.op("dve", lambda: V.tensor_tensor(out=cre[:, qs], in0=c1[:], in1=pwr[:, qs, Lc - 1], op=ALU.mult), [c1, pwr], [cre])
                    P.op("dve", lambda: V.tensor_tensor(out=cim[:, qs], in0=c1[:], in1=pwi[:, qs, Lc - 1], op=ALU.mult), [c1, pwi], [cim])
                    P.op("dve", lambda: V.tensor_tensor(out=c1[:], in0=c2[:], in1=pwi[:, qs, Lc - 1], op=ALU.mult), [c2, pwi], [c1])
                    P.op("dve", lambda: V.tensor_tensor(out=cre[:, qs], in0=cre[:, qs], in1=c1[:], op=ALU.subtract), [cre, c1], [cre])
                    P.op("dve", lambda: V.tensor_tensor(out=c1[:], in0=c2[:], in1=pwr[:, qs, Lc - 1], op=ALU.mult), [c2, pwr], [c1])
                    P.op("dve", lambda: V.tensor_tensor(out=cim[:, qs], in0=cim[:, qs], in1=c1[:], op=ALU.add), [cim, c1], [cim])
                    continue
                P.op("pool", lambda: G.tensor_tensor(out=Wr_[:, :, 0:Lc], in0=Wr_[:, :, 0:Lc], in1=cre[:, qs].unsqueeze(2).to_broadcast([128, 8, Lc]), op=ALU.add),
                     [Wr_, cre], [Wr_])
                P.op("pool", lambda: G.tensor_tensor(out=Wi_[:, :, 0:Lc], in0=Wi_[:, :, 0:Lc], in1=cim[:, qs].unsqueeze(2).to_broadcast([128, 8, Lc]), op=ALU.add),
                     [Wi_, cim], [Wi_])
                P.op("dve", lambda: V.tensor_tensor(out=sr32[:, :, 0:Lc], in0=Wr_[:, :, 0:Lc], in1=pwr[:, qs, 0:Lc], op=ALU.mult), [Wr_, pwr], [sr32])
                P.op("pool", lambda: G.tensor_tensor(out=tm1[:, :, 0:Lc], in0=Wi_[:, :, 0:Lc], in1=pwi[:, qs, 0:Lc], op=ALU.mult), [Wi_, pwi], [tm1])
                P.op("dve", lambda: V.tensor_tensor(out=sr32[:, :, 0:Lc], in0=sr32[:, :, 0:Lc], in1=tm1[:, :, 0:Lc], op=ALU.subtract), [sr32, tm1], [sr32])
                P.op("dve", lambda: V.tensor_tensor(out=si32[:, :, 0:Lc], in0=Wr_[:, :, 0:Lc], in1=pwi[:, qs, 0:Lc], op=ALU.mult), [Wr_, pwi], [si32])
                P.op("pool", lambda: G.tensor_tensor(out=tm2[:, :, 0:Lc], in0=Wi_[:, :, 0:Lc], in1=pwr[:, qs, 0:Lc], op=ALU.mult), [Wi_, pwr], [tm2])
                P.op("dve", lambda: V.tensor_tensor(out=si32[:, :, 0:Lc], in0=si32[:, :, 0:Lc], in1=tm2[:, :, 0:Lc], op=ALU.add), [si32, tm2], [si32])
                P.op("act", lambda: A.copy(out=srb[:, :, 0:Lc], in_=sr32[:, :, 0:Lc]), [sr32], [srb])
                P.op("act", lambda: A.copy(out=sib[:, :, 0:Lc], in_=si32[:, :, 0:Lc]), [si32], [sib])
                P.op("act", lambda: A.copy(out=cre[:, qs], in_=sr32[:, :, Lc - 1]), [sr32], [cre])
                P.op("act", lambda: A.copy(out=cim[:, qs], in_=si32[:, :, Lc - 1]), [si32], [cim])
                for k2 in range(2):
                    kc = 2 * qb + k2
                    psy = PS[2 + k2]
                    for p4 in range(4):
                        pi_ = 4 * k2 + p4
                        q = 8 * qb + pi_
                        P.mm(psy[:, 0:Lc], Cre[:, q, :], srb[:, pi_, 0:Lc], p4 == 0, False, [Cre, srb], [psy])
                        P.mm(psy[:, 0:Lc], Cim[:, q, :], sib[:, pi_, 0:Lc], False, p4 == 3, [Cim, sib], [psy])
                    P.op("dve", lambda: V.scalar_tensor_tensor(out=yv[:, 0:Lc], in0=uTb[:, kc, 0:Lc], scalar=dsk[:, kc:kc + 1], in1=psy[:, 0:Lc],
                                                               op0=ALU.mult, op1=ALU.add), [uTb, dsk, psy], [yv])
                    P.op("act", lambda: A.activation(out=y2[:, 0:Lc], in_=yv[:, 0:Lc], func=AF.Square), [yv], [y2])
                    P.op("dve", lambda: V.tensor_scalar(out=y2[:, 0:Lc], in0=y2[:, 0:Lc], scalar1=0.044715, scalar2=1.0, op0=ALU.mult, op1=ALU.add), [y2], [y2])
                    P.op("dve", lambda: V.tensor_tensor(out=y3[:, 0:Lc], in0=y2[:, 0:Lc], in1=yv[:, 0:Lc], op=ALU.mult), [y2, yv], [y3])
                    P.op("act", lambda: A.activation(out=y3[:, 0:Lc], in_=y3[:, 0:Lc], func=AF.Sigmoid, scale=1.5957691216057308), [y3], [y3])
                    P.op("dve", lambda: V.tensor_tensor(out=yo[:, kc, 0:Lc], in0=yv[:, 0:Lc], in1=y3[:, 0:Lc], op=ALU.mult), [yv, y3], [yo])
            if full:
                P.dma("sp", fm(yT)[:, :, col0:col0 + Lc], yo[:, :, 0:Lc], Rd=[yo], Wr=[P.R("yT5", col0)])

        def state_out5(dre, dim_):
            for src, dd in ((cre, dre), (cim, dim_)):
                transpose_to(ldt[:], ldt, src[:], src, 128, 64, PS[4])
                P.dma("sp", dd, ldt[:], Rd=[ldt], Wr=[P.R("os5", id(dd))])

        P.op("dve", lambda: V.memset(cre[:], 0.0), [], [cre]); P.op("dve", lambda: V.memset(cim[:], 0.0), [], [cim])
        for tb in range(NP // L5):
            scan_block(tb * L5, L5, False)
        P.dma("sp", xch5_in.ap()[:, 0:64], cre[:], Rd=[cre], Wr=[P.R("xch5")])
        P.dma("sp", xch5_in.ap()[:, 64:128], cim[:], Rd=[cim], Wr=[P.R("xch5")])
        P._deps("pool", [P.R("xch5")], [])
        ccs3 = nc.alloc_semaphore(name="ccsem3")
        nc.gpsimd.collective_compute("AllGather", ALU.bypass, replica_groups=[list(range(NCORES))],
                                     ins=[xch5_in.ap().opt()], outs=[xch5_out.ap().opt()]).then_inc(ccs3)
        for e_ in P.engs:
            P.engs[e_].wait_ge(ccs3, 1)
        S5A = sbp("S5A", [128, 8, 128]); Hr = sbp("Hr", [128, 64]); Hi = sbp("Hi", [128, 64]); Mr = sbp("Mr", [128, 64]); Mi = sbp("Mi", [128, 64])
        P.dma("sp", S5A[:], xch5_out.ap().rearrange("(r p) f -> p r f", p=128), Wr=[S5A])
        for t_ in (Hr, Hi, Mr, Mi):
            P.op("dve", lambda: V.memset(t_[:], 0.0), [], [t_])
        for k in range(NCORES):
            P.op("dve", lambda: V.scalar_tensor_tensor(out=Mr[:], in0=Hr[:], scalar=oneh[:, k:k + 1], in1=Mr[:], op0=ALU.mult, op1=ALU.add), [Hr, oneh, Mr], [Mr])
            P.op("dve", lambda: V.scalar_tensor_tensor(out=Mi[:], in0=Hi[:], scalar=oneh[:, k:k + 1], in1=Mi[:], op0=ALU.mult, op1=ALU.add), [Hi, oneh, Mi], [Mi])
            a1, a2, a3 = t64[3], t64[4], t64[5]
            P.op("dve", lambda: V.tensor_tensor(out=a1[:], in0=Hr[:], in1=Ar[:], op=ALU.mult), [Hr, Ar], [a1])
            P.op("dve", lambda: V.tensor_tensor(out=a2[:], in0=Hi[:], in1=Ai[:], op=ALU.mult), [Hi, Ai], [a2])
            P.op("dve", lambda: V.tensor_tensor(out=a1[:], in0=a1[:], in1=a2[:], op=ALU.subtract), [a1, a2], [a1])
            P.op("dve", lambda: V.tensor_tensor(out=a2[:], in0=Hr[:], in1=Ai[:], op=ALU.mult), [Hr, Ai], [a2])
            P.op("dve", lambda: V.tensor_tensor(out=a3[:], in0=Hi[:], in1=Ar[:], op=ALU.mult), [Hi, Ar], [a3])
            P.op("dve", lambda: V.tensor_tensor(out=a2[:], in0=a2[:], in1=a3[:], op=ALU.add), [a2, a3], [a2])
            P.op("dve", lambda: V.tensor_tensor(out=Hr[:], in0=a1[:], in1=S5A[:, k, 0:64], op=ALU.add), [a1, S5A], [Hr])
            P.op("dve", lambda: V.tensor_tensor(out=Hi[:], in0=a2[:], in1=S5A[:, k, 64:128], op=ALU.add), [a2, S5A], [Hi])
        P.op("dve", lambda: V.tensor_copy(out=cre[:], in_=Hr[:]), [Hr], [cre]); P.op("dve", lambda: V.tensor_copy(out=cim[:], in_=Hi[:]), [Hi], [cim])
        state_out5(o_s5re, o_s5im)
        P.op("dve", lambda: V.tensor_copy(out=cre[:], in_=Mr[:]), [Mr], [cre]); P.op("dve", lambda: V.tensor_copy(out=cim[:], in_=Mi[:]), [Mi], [cim])
        for tb in range(NP // L5):
            scan_block(tb * L5, L5, True)
        for src, dst in ((s5re0, cre), (s5im0, cim)):
            P.dma("sp", ldt[:], src, Wr=[ldt])
            transpose_to(dst[:], dst, ldt[:], ldt, 64, 128, PS[4])
        scan_block(NP, NS, True)
        state_out5(o_s5res, o_s5ims)

    if STOP >= 7:
        with ExitStack() as es:
            s5(es)
            P.barrier()

    esC = ExitStack()
    if STOP >= 8:
        xg, hT, wts, xin, xst, st_bf, st_f32 = mk_dense(esC, "dC", with_x=False)
        xin = [TL(esC.enter_context(nc.sbuf_tensor("dC_xin0", [128, D], F32)))]
    if STOP >= 8:
        with ExitStack() as es:
            sbp = dense_bufs(es, "p8")
            sbx = mk_sbx(sbp)
            sgt = [sbp("sgt0", [128, 528]), sbp("sgt1", [128, 528])]
            for grp in groups:
                sg, n, c0 = load_x(xT[0], grp, 2, want_h=False)
                P.dma("sp", hT[:, :, 0:n], fm(yT)[:, :, c0:c0 + n], Wr=[hT])
                rT = sbx["rT"]

                def ev_glu(mc, res):
                    t_ = sgt[mc % 2]
                    for (psv, o, m, s), (psg, _, _, _) in zip(res[0], res[1]):
                        P.op("act", lambda: A.activation(out=t_[:, o:o + m], in_=psg[:, 0:m], func=AF.Sigmoid), [psg], [t_])
                        P.op("dve", lambda: V.tensor_tensor(out=t_[:, o:o + m], in0=t_[:, o:o + m], in1=psv[:, 0:m], op=ALU.mult), [t_, psv], [t_])
                        P.op("dve", lambda: V.scalar_tensor_tensor(out=rT[:, mc, o:o + m], in0=t_[:, o:o + m], scalar=g1[:, 2, s, mc:mc + 1], in1=xg[:, mc, o:o + m],
                                                                   op0=ALU.mult, op1=ALU.add), [t_, g1, xg], [rT])
                proj_fm([glu_w[:, 0:D], glu_w[:, D:2 * D]], 16, D, hT, sg, ev_glu, 512)
                ln_group(sbx, 2, n, sg, store_xT(xT[1], c0, n))
            P.barrier()

    if STOP >= 9:
        with ExitStack() as es:
            sbp = dense_bufs(es, "p9")
            sbx = mk_sbx(sbp)
            rT = sbx["rT"]
            h32k = [sbp("h32k0", [128, 528]), sbp("h32k1", [128, 528])]; hid = sbp("hid", [128, 22, 528], BF16)
            sgt = [sbp("sgt0", [128, 528]), sbp("sgt1", [128, 528])]
            wr32 = sbp("wr32", [128, 16, 8]); rbb = sbp("rbb", [128, 1, 8]); sel = sbp("sel", [8, 1024])
            lg = sbp("lg", [128, 8]); mx8 = sbp("mx8", [128, 8]); w1 = sbp("w1", [128, 1]); w2 = sbp("w2", [128, 1])
            ga = sbp("ga", [128, 8]); gb_ = sbp("gb", [128, 8]); gT = sbp("gT", [8, 528]); Gbe = sbp("Gbe", [128, 528]); tmpm = sbp("tmpm", [128, 528])
            P.dma("sp", wr32[:], router_w.rearrange("(kc p) e -> p kc e", p=128), Wr=[wr32])
            P.dma("sp", rbb[:], router_b.partition_broadcast(128), Wr=[rbb])
            P.dma("sp", sel[:], sel_d, Wr=[sel])
            for gi, grp in enumerate(groups):
                sg, n, c0 = segs(grp)[0], segs(grp)[1], grp[0][0]
                P.dma("sp", xg[:, :, 0:n], fm(xT[1])[:, :, c0:c0 + n], Wr=[xg])
                for kc in range(KC):
                    hk = h32k[kc % 2]
                    for (o, m, s) in sg:
                        P.op("act", lambda: A.activation(out=hT[:, kc, o:o + m], in_=xg[:, kc, o:o + m], func=AF.Identity,
                                                         scale=sc1[:, 3, s, kc:kc + 1], bias=mod[:, 3, s, kc:kc + 1]), [xg, sc1, mod], [hT])
                        P.op("dve", lambda: V.tensor_scalar(out=hk[:, o:o + m], in0=xg[:, kc, o:o + m], scalar1=sc1[:, 3, s, kc:kc + 1],
                                                            scalar2=mod[:, 3, s, kc:kc + 1], op0=ALU.mult, op1=ALU.add), [xg, sc1, mod], [hk])
                    bo = 0
                    for bi3, (cb, nb, sq_) in enumerate(grp):
                        P.mm(PS[bi3][0:nb, 0:8], hk[:, bo:bo + nb], wr32[:, kc, :], kc == 0, kc == KC - 1, [hk, wr32], [PS[bi3]])
                        bo += nb
                P.op("pool", lambda: G.tensor_scalar(out=xg[:, :, 0:n], in0=xg[:, :, 0:n], scalar1=ALPHA, scalar2=None, op0=ALU.mult), [xg], [xg])
                bo = 0
                for bi3, (cb, nb, sq_) in enumerate(grp):
                    P.op("dve", lambda: V.tensor_tensor(out=lg[0:nb, :], in0=PS[bi3][0:nb, 0:8], in1=rbb[0:nb, 0, :], op=ALU.add), [PS[bi3], rbb], [lg])
                    P.op("dve", lambda: V.max(out=mx8[0:nb, :], in_=lg[0:nb, :]), [lg], [mx8])
                    P.op("dve", lambda: V.tensor_tensor(out=w2[0:nb, :], in0=mx8[0:nb, 1:2], in1=mx8[0:nb, 0:1], op=ALU.subtract), [mx8], [w2])
                    P.op("act", lambda: A.activation(out=w2[0:nb, :], in_=w2[0:nb, :], func=AF.Exp), [w2], [w2])
                    P.op("dve", lambda: V.tensor_scalar(out=w1[0:nb, :], in0=w2[0:nb, :], scalar1=1.0, scalar2=None, op0=ALU.add), [w2], [w1])
                    P.op("dve", lambda: V.reciprocal(out=w1[0:nb, :], in_=w1[0:nb, :]), [w1], [w1])
                    P.op("dve", lambda: V.tensor_tensor(out=w2[0:nb, :], in0=w2[0:nb, :], in1=w1[0:nb, :], op=ALU.mult), [w2, w1], [w2])
                    P.op("dve", lambda: V.tensor_scalar(out=ga[0:nb, :], in0=lg[0:nb, :], scalar1=mx8[0:nb, 0:1], scalar2=w1[0:nb, 0:1], op0=ALU.is_equal, op1=ALU.mult),
                         [lg, mx8, w1], [ga])
                    P.op("dve", lambda: V.tensor_scalar(out=gb_[0:nb, :], in0=lg[0:nb, :], scalar1=mx8[0:nb, 1:2], scalar2=w2[0:nb, 0:1], op0=ALU.is_equal, op1=ALU.mult),
                         [lg, mx8, w2], [gb_])
                    P.op("dve", lambda: V.tensor_tensor(out=ga[0:nb, :], in0=ga[0:nb, :], in1=gb_[0:nb, :], op=ALU.add), [ga, gb_], [ga])
                    transpose_to(gT[0:8, bo:bo + nb], gT, ga[0:nb, :], ga, nb, 8, PS[5])
                    bo += nb
                for e in range(8):
                    for si, (o, m, s) in enumerate(sg):
                        P.mm(PS[2 + si][:, 0:m], sel[0:8, e * 128:(e + 1) * 128], gT[0:8, o:o + m], True, True, [sel, gT], [PS[2 + si]])
                        P.op("act", lambda: A.copy(out=Gbe[:, o:o + m], in_=PS[2 + si][:, 0:m]), [PS[2 + si]], [Gbe])

                    def ev_up(mc, res):
                        t_ = sgt[mc % 2]
                        for (psg, o, m, s), (psu, _, _, _) in zip(res[0], res[1]):
                            P.op("act", lambda: A.activation(out=t_[:, o:o + m], in_=psg[:, 0:m], func=AF.Silu), [psg], [t_])
                            P.op("dve", lambda: V.tensor_tensor(out=hid[:, mc, o:o + m], in0=t_[:, o:o + m], in1=psu[:, 0:m], op=ALU.mult), [t_, psu], [hid])
                    proj_fm([moe_up[e][:, 0:2816], moe_up[e][:, 2816:5632]], 16, 2816, hT, sg, ev_up, 256)

                    def ev_dn(mc, res):
                        for (ps, o, m, s) in res[0]:
                            if e == 0:
                                P.op("dve", lambda: V.tensor_tensor(out=rT[:, mc, o:o + m], in0=ps[:, 0:m], in1=Gbe[:, o:o + m], op=ALU.mult), [ps, Gbe], [rT])
                            else:
                                P.op("dve", lambda: V.tensor_tensor(out=tmpm[:, o:o + m], in0=ps[:, 0:m], in1=Gbe[:, o:o + m], op=ALU.mult), [ps, Gbe], [tmpm])
                                P.op("pool", lambda: G.tensor_tensor(out=rT[:, mc, o:o + m], in0=rT[:, mc, o:o + m], in1=tmpm[:, o:o + m], op=ALU.add), [rT, tmpm], [rT])
                    proj_fm([moe_dn[e]], 22, D, hid, sg, ev_dn, 256)
                for kc in range(KC):
                    for (o, m, s) in sg:
                        P.op("dve", lambda: V.scalar_tensor_tensor(out=rT[:, kc, o:o + m], in0=rT[:, kc, o:o + m], scalar=g1[:, 3, s, kc:kc + 1], in1=xg[:, kc, o:o + m],
                                                                   op0=ALU.mult, op1=ALU.add), [rT, g1, xg], [rT])

                def store_out():
                    bo2 = 0
                    for bi2, (cb, nb, sq_) in enumerate(grp):
                        xi = xin[0]
                        for q4 in range(4):
                            ps = PS[q4 % 4]
                            for j in range(4):
                                kc = q4 * 4 + j
                                P.op("pe", lambda: PE.transpose(out=ps[0:nb, j * 128:(j + 1) * 128], in_=xg[:, kc, bo2:bo2 + nb], identity=ident[:, :]), [xg, ident], [ps])
                            P.op("act" if q4 % 2 else "dve",
                                 (lambda: A.copy(out=xi[0:nb, q4 * 512:(q4 + 1) * 512], in_=ps[0:nb, :])) if q4 % 2 else
                                 (lambda: V.tensor_copy(out=xi[0:nb, q4 * 512:(q4 + 1) * 512], in_=ps[0:nb, :])), [ps], [xi])
                        dst = o_ys[:, :] if sq_ else o_y[cb:cb + nb, :]
                        P.dma("sp", dst, xi[0:nb, :], Rd=[xi], Wr=[P.R("oy", cb, sq_)])
                        bo2 += nb
                ln_group(sbx, 3, n, sg, store_out)
            P.barrier()

    esC.close()

    P.barrier()
    P.finish()
    return P


_CACHE = {}


def _consts():
    ident = np.eye(128, dtype=np.float32)
    j = np.arange(128)
    triu = (j[:, None] <= j[None, :]).astype(np.float32)
    mneg = np.where(j[:, None] <= j[None, :], 0.0, NEG).astype(np.float32)
    sel = np.zeros((8, 8, 128), np.float32)
    for e in range(8):
        sel[e, e, :] = 1.0
    segm = np.ones((128, 512), np.float32)
    segm[:, ::64] = 0.0
    return dict(ident=ident, triu=triu, mneg=mneg, sel=sel.reshape(8, 1024), segm=segm)


def kernel(**inp):
    f = lambda k: np.ascontiguousarray(np.asarray(inp[k], dtype=np.float32))
    x_prompt = f("x_prompt")[0]; x_sample = f("x_sample")
    if "P" not in _CACHE:
        _CACHE["P"] = build()
    P = _CACHE["P"]
    cst = _consts()
    rb = f("rel_bias")[0]
    q = np.arange(128)[:, None]; kk = np.arange(640)[None, :]
    idx = np.clip(q + 512 - kk, -128, 128) + 128
    tblp = np.ascontiguousarray(rb[:, idx].transpose(1, 0, 2))
    qi = np.arange(16)[:, None]; ks = np.arange(528)[None, :]
    dif = np.where(ks < 512, 512 + qi - ks, qi - (ks - 512))
    idxs = np.clip(dif, -128, 128) + 128
    tbls = np.ascontiguousarray(rb[:, idxs].transpose(1, 0, 2))
    b_re = f("s5_b_re")[0]; b_im = f("s5_b_im")[0]; c_re = f("s5_c_re")[0]; c_im = f("s5_c_im")[0]

    def bpad(b):
        out = np.zeros((128, 64, 128), np.float32)
        for g in range(128):
            qq, gi, g8 = g // 2, g % 2, g % 8
            out[g8 * 16:(g8 + 1) * 16, qq, gi * 64:(gi + 1) * 64] = b[g].T
        return out

    def cpad(c):
        out = np.zeros((128, 64, 128), np.float32)
        for g in range(128):
            qq, gi, g8 = g // 2, g % 2, g % 8
            out[gi * 64:(gi + 1) * 64, qq, g8 * 16:(g8 + 1) * 16] = c[g].T
        return out

    units = {}
    aw = f("ada_w").reshape(4, D, 3 * D)
    for a_ in range(4):
        units[f"ada_w{a_}"] = aw[a_]
    units["glu_w"] = f("glu_w")[0]; units["w_out0"] = f("w_out0")[0]; units["w_in1"] = f("w_in1")[0]
    mu = f("moe_w_up")[0]; md = f("moe_w_down")[0]
    for e_ in range(8):
        units[f"moe_up{e_}"] = mu[e_]; units[f"moe_dn{e_}"] = md[e_]
    fu = f("ffn_w_up")[0]
    units["ffn_g"] = fu[:, :5632]; units["ffn_u"] = fu[:, 5632:]; units["ffn_dn"] = f("ffn_w_down")[0]
    units["w_in0"] = f("w_in0")[0]
    units["s5bre"] = bpad(b_re); units["s5bim"] = bpad(b_im); units["s5cre"] = cpad(c_re); units["s5cim"] = cpad(c_im)
    units["tblp"] = tblp
    chunks = np.zeros((NCHUNK, CH_ROWS, 2048), np.float32)
    for name, (c_, r0, shp) in WPLACE.items():
        arr = np.ascontiguousarray(units[name], dtype=np.float32).reshape(-1, 2048)
        chunks[c_, r0:r0 + arr.shape[0]] = arr
    shared = dict(
        cp=f("c_prompt").reshape(16, 128), ada_b=f("ada_b").reshape(192, 128),
        ln_g=f("ln_g").reshape(64, 128), ln_b=f("ln_b").reshape(64, 128), tbls=tbls,
        conv_w=f("conv_w")[0], conv_b=f("conv_b"), dt_bias=f("dt_bias"), a_log=f("a_log"), ssd_d=f("ssd_d"),
        ssd_ng=f("ssd_norm_g"),
        lamre=f("s5_lam_re")[0].reshape(64, 128), lamim=f("s5_lam_im")[0].reshape(64, 128),
        lstep=f("s5_log_step")[0].reshape(64, 2),
        s5d=f("s5_d").reshape(16, 128),
        router_w=f("router_w")[0], router_b=f("router_b"),
        **cst)
    in_maps = []
    for c in range(NCORES):
        m = dict(shared)
        m["wsh"] = np.ascontiguousarray(chunks[:, c * (CH_ROWS // NCORES):(c + 1) * (CH_ROWS // NCORES), :])
        m["xp"] = x_prompt[c * NP:(c + 1) * NP]
        m["xh"] = x_prompt[c * NP - NH:c * NP] if c > 0 else np.zeros((NH, D), np.float32)
        m["xs"] = x_sample[c]
        m["cs"] = f("c_sample")[c].reshape(16, 128)
        m["ck"] = f("cache_attn_k")[0, c].reshape(512, 1024); m["cv"] = f("cache_attn_v")[0, c].reshape(512, 1024)
        m["sconv"] = f("state_ssd_conv")[0, c]; m["sssd"] = f("state_ssd")[0, c].reshape(1024, 128)
        m["s5re0"] = f("state_s5_re")[0, c].reshape(64, 128); m["s5im0"] = f("state_s5_im")[0, c].reshape(64, 128)
        m["flag"] = np.full((128, 1), 0.0 if c == 0 else 1.0, np.float32)
        m["pen"] = np.full((128, 1), NEG if c == 0 else 0.0, np.float32)
        oh = np.zeros((128, 8), np.float32); oh[:, c] = 1.0
        m["oneh"] = oh
        in_maps.append(m)
    res = run_bass_kernel_spmd(P.nc, in_maps, core_ids=list(range(NCORES)))
    R = res.results
    y_p = np.concatenate([R[c]["o_y"] for c in range(NCORES)], 0)[None]
    y_s = np.stack([R[c]["o_ys"] for c in range(NCORES)], 0)
    k_p = R[7]["o_k"].reshape(1, 1, 512, 16, 64); v_p = R[7]["o_v"].reshape(1, 1, 512, 16, 64)
    conv_p = R[7]["o_conv"].reshape(1, 1, 3, 1536)
    ssd_p = R[0]["o_ssd"].reshape(1, 1, 16, 64, 128)
    s5re_p = R[0]["o_s5re"].reshape(1, 1, 128, 64); s5im_p = R[0]["o_s5im"].reshape(1, 1, 128, 64)
    k_s = np.stack([R[c]["o_ks"] for c in range(NCORES)], 0).reshape(1, 8, 16, 16, 64)
    v_s = np.stack([R[c]["o_vs"] for c in range(NCORES)], 0).reshape(1, 8, 16, 16, 64)
    conv_s = np.stack([R[c]["o_convs"] for c in range(NCORES)], 0).reshape(1, 8, 3, 1536)
    ssd_s = np.stack([R[c]["o_ssds"] for c in range(NCORES)], 0).reshape(1, 8, 16, 64, 128)
    s5re_s = np.stack([R[c]["o_s5res"] for c in range(NCORES)], 0).reshape(1, 8, 128, 64)
    s5im_s = np.stack([R[c]["o_s5ims"] for c in range(NCORES)], 0).reshape(1, 8, 128, 64)
    outs = (y_p, y_s, k_p, v_p, conv_p, ssd_p, s5re_p, s5im_p, k_s, v_s, conv_s, ssd_s, s5re_s, s5im_s)
    return tuple(np.ascontiguousarray(o, dtype=np.float32) for o in outs)
```
